# Optimizing a Trainium2 kernel written in Bass

```python
import math
import jax, jax.numpy as jnp
from jax import lax
import numpy as np

D_MODEL = 2048
BATCH = 2
SEQ = 16384
DEPTH = 2

N_EVEN = (DEPTH + 1) // 2
N_ODD = DEPTH // 2

DN_ALPHA = (2.0 * DEPTH) ** 0.25
DN_BETA = (8.0 * DEPTH) ** -0.25
LN_EPS = 1e-5

CS_BLOCK = 128

POOL_WIDTH = D_MODEL // 2
POOL_WINDOWS = (2, 4, 8, 16)
POOL_GROUPS = len(POOL_WINDOWS)
POOL_GROUP_DIM = POOL_WIDTH // POOL_GROUPS

SB_HEADS = 8
SB_HEAD_DIM = (D_MODEL - POOL_WIDTH) // SB_HEADS
SB_WIDTH = SB_HEADS * SB_HEAD_DIM
SB_BLOCK = 128

EVEN_IN = POOL_WIDTH + 3 * SB_WIDTH
EVEN_MIX = POOL_WIDTH + SB_WIDTH

SGU_WIDTH = D_MODEL
SGU_GROUPS = 8
SGU_GROUP_DIM = SGU_WIDTH // SGU_GROUPS
SGU_CHUNK = 128
ODD_IN = 2 * SGU_WIDTH

FFN_DIM = 5632
N_EXPERTS = 8
TOP_K = 2
EXPERT_DIM = 1408

kernel_name = 'hybrid_pool_stickbreak_sgu_moe_deepnorm'


def layer_norm(x, g, b):
    xf = x.astype(jnp.float32)
    mu = jnp.mean(xf, axis=-1, keepdims=True)
    xc = xf - mu
    var = jnp.mean(xc * xc, axis=-1, keepdims=True)
    y = xc * lax.rsqrt(var + LN_EPS)
    return (y * g.astype(jnp.float32) + b.astype(jnp.float32)).astype(x.dtype)


def block_cumsum(x):
    lead = x.shape[:-1]
    L = x.shape[-1]
    nb = L // CS_BLOCK
    xb = x.astype(jnp.float32).reshape(lead + (nb, CS_BLOCK))
    tri = jnp.triu(jnp.ones((CS_BLOCK, CS_BLOCK), jnp.float32))
    within = jnp.matmul(xb, tri, precision=lax.Precision.HIGHEST)
    totals = within[..., -1]
    stri = jnp.triu(jnp.ones((nb, nb), jnp.float32), k=1)
    prefix = jnp.matmul(totals, stri, precision=lax.Precision.HIGHEST)
    return (within + prefix[..., None]).reshape(lead + (L,))


def swiglu(x, w_gu, w_down):
    gate, up = jnp.split(x @ w_gu, 2, axis=-1)
    return (jax.nn.silu(gate) * up) @ w_down


def pool_mixer(p, pool_w, pool_scale):
    B, S, _ = p.shape
    pf = p.astype(jnp.float32)
    cs = jnp.swapaxes(block_cumsum(jnp.swapaxes(pf, 1, 2)), 1, 2)
    pos = jnp.arange(S)
    outs = []
    for g, w in enumerate(POOL_WINDOWS):
        sl = slice(g * POOL_GROUP_DIM, (g + 1) * POOL_GROUP_DIM)
        c = cs[..., sl]
        lagged = jnp.pad(c, ((0, 0), (w, 0), (0, 0)))[:, :S]
        count = jnp.minimum(pos + 1, w).astype(jnp.float32)[None, :, None]
        outs.append((c - lagged) / count - pf[..., sl])
    d = jnp.stack(outs, axis=2).astype(p.dtype)
    y = jnp.einsum('bsgc,gcd->bsgd', d, pool_w).reshape(B, S, POOL_WIDTH)
    return y * pool_scale


def stick_breaking_attention(q, k, v):
    B, S, H, Dh = q.shape
    qh = jnp.transpose(q, (0, 2, 1, 3))
    kh = jnp.transpose(k, (0, 2, 1, 3))
    vh = jnp.transpose(v, (0, 2, 1, 3))
    n_blocks = S // SB_BLOCK
    scale = 1.0 / math.sqrt(Dh)
    outs = []
    for i in range(n_blocks):
        start = i * SB_BLOCK
        kv_len = start + SB_BLOCK
        qb = qh[:, :, start:kv_len]
        kb = kh[:, :, :kv_len]
        vb = vh[:, :, :kv_len]
        z = jnp.einsum('bhqd,bhkd->bhqk', qb, kb,
                       preferred_element_type=jnp.float32) * scale
        q_pos = start + jnp.arange(SB_BLOCK)
        causal = jnp.arange(kv_len)[None, :] < q_pos[:, None]
        log_not = jnp.where(causal, jax.nn.log_sigmoid(-z), 0.0)
        c = block_cumsum(log_not)
        between = c[..., -1:] - c
        a = jnp.where(causal, jnp.exp(jax.nn.log_sigmoid(z) + between), 0.0)
        outs.append(jnp.einsum('bhqk,bhkd->bhqd', a.astype(vb.dtype), vb))
    o = jnp.concatenate(outs, axis=2)
    return jnp.transpose(o, (0, 2, 1, 3)).reshape(B, S, H * Dh)


def spatial_gating(z, ln_g, ln_b, w_s, b_s):
    B, S, _ = z.shape
    z = jax.nn.gelu(z)
    u, v = jnp.split(z, 2, axis=-1)
    v = layer_norm(v, ln_g, ln_b)
    v = v.reshape(B, S // SGU_CHUNK, SGU_CHUNK, SGU_GROUPS, SGU_GROUP_DIM)
    mask = jnp.tril(jnp.ones((SGU_CHUNK, SGU_CHUNK), dtype=bool))
    w = jnp.where(mask[None], w_s, 0.0).astype(v.dtype)
    mixed = jnp.einsum('gts,bnsgc->bntgc', w, v) + jnp.transpose(b_s)[None, None, :, :, None]
    return u * mixed.reshape(B, S, SGU_WIDTH)


def moe_swiglu(x, w_router, w_gu, w_down):
    B, S, Dm = x.shape
    xt = x.reshape(B * S, Dm)
    logits = jnp.dot(xt, w_router, preferred_element_type=jnp.float32)
    top_vals, top_idx = lax.top_k(logits, TOP_K)
    top_w = jax.nn.softmax(top_vals, axis=-1)
    gates = jnp.sum(jax.nn.one_hot(top_idx, N_EXPERTS, dtype=jnp.float32)
                    * top_w[..., None], axis=1)
    y = jnp.zeros(xt.shape, jnp.float32)
    for e in range(N_EXPERTS):
        y = y + gates[:, e:e + 1] * swiglu(xt, w_gu[e], w_down[e])
    return y.astype(x.dtype).reshape(B, S, Dm)


def _normal(key, shape, scale):
    return jax.random.normal(key, shape, jnp.float32) * scale


def setup_inputs(seed: int = 0) -> dict:
    key = jax.random.key(seed)
    ks = jax.random.split(key, 32)
    D = D_MODEL
    return {
        'x': _normal(ks[0], (BATCH, SEQ, D), 1.0),
        'even_w_in': _normal(ks[1], (N_EVEN, D, EVEN_IN), D ** -0.5),
        'even_pool_w': _normal(ks[2], (N_EVEN, POOL_GROUPS, POOL_GROUP_DIM, POOL_GROUP_DIM), POOL_GROUP_DIM ** -0.5),
        'even_pool_scale': 1.0 + _normal(ks[3], (N_EVEN, POOL_WIDTH), 0.02),
        'even_w_out': _normal(ks[4], (N_EVEN, EVEN_MIX, D), DN_BETA * EVEN_MIX ** -0.5),
        'even_ln1_g': 1.0 + _normal(ks[5], (N_EVEN, D), 0.02),
        'even_ln1_b': _normal(ks[6], (N_EVEN, D), 0.02),
        'even_ffn_w_gu': _normal(ks[7], (N_EVEN, D, 2 * FFN_DIM), D ** -0.5),
        'even_ffn_w_down': _normal(ks[8], (N_EVEN, FFN_DIM, D), DN_BETA * FFN_DIM ** -0.5),
        'even_ln2_g': 1.0 + _normal(ks[9], (N_EVEN, D), 0.02),
        'even_ln2_b': _normal(ks[10], (N_EVEN, D), 0.02),
        'odd_w_in': _normal(ks[11], (N_ODD, D, ODD_IN), D ** -0.5),
        'odd_sgu_ln_g': 1.0 + _normal(ks[12], (N_ODD, SGU_WIDTH), 0.02),
        'odd_sgu_ln_b': _normal(ks[13], (N_ODD, SGU_WIDTH), 0.02),
        'odd_sgu_w': _normal(ks[14], (N_ODD, SGU_GROUPS, SGU_CHUNK, SGU_CHUNK), SGU_CHUNK ** -0.5),
        'odd_sgu_b': 1.0 + _normal(ks[15], (N_ODD, SGU_GROUPS, SGU_CHUNK), 0.02),
        'odd_w_out': _normal(ks[16], (N_ODD, SGU_WIDTH, D), DN_BETA * SGU_WIDTH ** -0.5),
        'odd_ln1_g': 1.0 + _normal(ks[17], (N_ODD, D), 0.02),
        'odd_ln1_b': _normal(ks[18], (N_ODD, D), 0.02),
        'odd_router': _normal(ks[19], (N_ODD, D, N_EXPERTS), D ** -0.5),
        'odd_moe_w_gu': _normal(ks[20], (N_ODD, N_EXPERTS, D, 2 * EXPERT_DIM), D ** -0.5),
        'odd_moe_w_down': _normal(ks[21], (N_ODD, N_EXPERTS, EXPERT_DIM, D), DN_BETA * EXPERT_DIM ** -0.5),
        'odd_ln2_g': 1.0 + _normal(ks[22], (N_ODD, D), 0.02),
        'odd_ln2_b': _normal(ks[23], (N_ODD, D), 0.02),
    }


def reference(x, even_w_in, even_pool_w, even_pool_scale, even_w_out, even_ln1_g, even_ln1_b,
              even_ffn_w_gu, even_ffn_w_down, even_ln2_g, even_ln2_b,
              odd_w_in, odd_sgu_ln_g, odd_sgu_ln_b, odd_sgu_w, odd_sgu_b, odd_w_out,
              odd_ln1_g, odd_ln1_b, odd_router, odd_moe_w_gu, odd_moe_w_down,
              odd_ln2_g, odd_ln2_b):
    B, S, _ = x.shape
    h = x
    for layer in range(DEPTH):
        i = layer // 2
        if layer % 2 == 0:
            proj = h @ even_w_in[i]
            p = proj[..., :POOL_WIDTH]
            q, k, v = jnp.split(proj[..., POOL_WIDTH:], 3, axis=-1)
            q = q.reshape(B, S, SB_HEADS, SB_HEAD_DIM)
            k = k.reshape(B, S, SB_HEADS, SB_HEAD_DIM)
            v = v.reshape(B, S, SB_HEADS, SB_HEAD_DIM)
            mixed = jnp.concatenate(
                [pool_mixer(p, even_pool_w[i], even_pool_scale[i]),
                 stick_breaking_attention(q, k, v)], axis=-1)
            h = layer_norm(DN_ALPHA * h + mixed @ even_w_out[i], even_ln1_g[i], even_ln1_b[i])
            h = layer_norm(DN_ALPHA * h + swiglu(h, even_ffn_w_gu[i], even_ffn_w_down[i]),
                           even_ln2_g[i], even_ln2_b[i])
        else:
            z = h @ odd_w_in[i]
            mixed = spatial_gating(z, odd_sgu_ln_g[i], odd_sgu_ln_b[i], odd_sgu_w[i], odd_sgu_b[i])
            h = layer_norm(DN_ALPHA * h + mixed @ odd_w_out[i], odd_ln1_g[i], odd_ln1_b[i])
            h = layer_norm(DN_ALPHA * h + moe_swiglu(h, odd_router[i], odd_moe_w_gu[i], odd_moe_w_down[i]),
                           odd_ln2_g[i], odd_ln2_b[i])
    return h
```

```python
from contextlib import ExitStack
import numpy as np
import concourse.bass as bass
import concourse.mybir as mybir
from concourse.bass_utils import run_bass_kernel_spmd


F32 = mybir.dt.float32
F32R = mybir.dt.float32r
BF16 = mybir.dt.bfloat16
AF = mybir.ActivationFunctionType
ALU = mybir.AluOpType


class T:
    __slots__ = ("name", "w", "r", "dsem")

    def __init__(self, name=""):
        self.name = name
        self.w = None
        self.r = {}
        self.dsem = None


class DSem:
    def __init__(self, S, name):
        self.key = ("d", name)
        self.sem = S.nc.alloc_semaphore("d_" + name)
        self.val = 0
        S.semobj[self.key] = self.sem
        S.dsems.append(self)


class S:
    ENG = ("pe", "act", "dve", "pool", "sp")

    def __init__(self, nc, same_engine_sync=True):
        self.nc = nc
        self.eng = {"pe": nc.tensor, "act": nc.scalar, "dve": nc.vector, "pool": nc.gpsimd, "sp": nc.sync}
        self.semobj = {}
        self.cnt = {}
        self.known = {}
        for e in self.ENG:
            self.semobj[e] = nc.alloc_semaphore("s_" + e)
            self.cnt[e] = 0
            self.known[e] = {}
        self.snap = {}
        self.dsems = []
        self.same = same_engine_sync
        self.nwait = 0
        self.ninst = 0
        self._dn = 0

    def dsem(self, name=None):
        self._dn += 1
        return DSem(self, name or f"x{self._dn}")

    def need(self, e, reads=(), writes=()):
        req = {}
        for t in reads:
            if t.w is not None:
                k, v = t.w
                if req.get(k, 0) < v:
                    req[k] = v
        for t in writes:
            if t.w is not None:
                k, v = t.w
                if req.get(k, 0) < v:
                    req[k] = v
            for k, v in t.r.items():
                if req.get(k, 0) < v:
                    req[k] = v
        kn = self.known[e]
        for k, v in req.items():
            if kn.get(k, 0) >= v:
                continue
            if k == e and not self.same:
                continue
            self.eng[e].wait_ge(self.semobj[k], v)
            self.nwait += 1
            if kn.get(k, 0) < v:
                kn[k] = v
            sn = self.snap.get((k, v))
            if sn:
                for k2, v2 in sn.items():
                    if kn.get(k2, 0) < v2:
                        kn[k2] = v2

    def done(self, e, ins, reads=(), writes=()):
        self.cnt[e] += 1
        ins.then_inc(self.semobj[e], 1)
        ev = (e, self.cnt[e])
        self.snap[ev] = dict(self.known[e])
        for t in reads:
            if t.r.get(e, 0) < ev[1]:
                t.r[e] = ev[1]
        for t in writes:
            t.w = ev
            t.r = {}
        self.ninst += 1
        return ev

    def op(self, e, fn, reads=(), writes=()):
        self.need(e, reads, writes)
        ins = fn()
        return self.done(e, ins, reads, writes)

    def dma(self, q, out_ap, in_ap, reads=(), writes=(), dsem=None):
        self.need(q, reads, writes)
        if dsem is None:
            t = (list(writes) + list(reads))[0]
            if t.dsem is None:
                t.dsem = self.dsem(t.name or None)
            dsem = t.dsem
        dsem.val += 16
        self.eng[q].dma_start(out=out_ap, in_=in_ap).then_inc(dsem.sem, 16)
        ev = (dsem.key, dsem.val)
        self.snap[ev] = dict(self.known[q])
        for t in reads:
            t.r[dsem.key] = dsem.val
        for t in writes:
            t.w = ev
            t.r = {}
        self.ninst += 1
        return ev

    def dma_multi(self, q, pairs, reads=(), writes=(), dsem=None):
        self.need(q, reads, writes)
        for out_ap, in_ap in pairs:
            dsem.val += 16
            self.eng[q].dma_start(out=out_ap, in_=in_ap).then_inc(dsem.sem, 16)
            self.ninst += 1
        ev = (dsem.key, dsem.val)
        self.snap[ev] = dict(self.known[q])
        for t in reads:
            t.r[dsem.key] = dsem.val
        for t in writes:
            t.w = ev
            t.r = {}
        return ev

    def barrier(self):
        keys = {e: self.cnt[e] for e in self.ENG}
        for d in self.dsems:
            keys[d.key] = d.val
        for e in self.ENG:
            kn = self.known[e]
            for k, v in keys.items():
                if v > 0 and kn.get(k, 0) < v:
                    self.eng[e].wait_ge(self.semobj[k], v)
                    self.nwait += 1
                    kn[k] = v

    def wait_all_dma(self, q, dsems):
        for d in dsems:
            if d.val > 0:
                self.eng[q].wait_ge(d.sem, d.val)


DH = 128
QT = 512


def declare_attn_io(nc, S_len, NH=2):
    ioa = {}
    ioa["xT"] = nc.dram_tensor("xT", [D, S_len], F32R, kind="ExternalInput").ap()
    ioa["wqkv"] = nc.dram_tensor("wqkv", [NH, 128, KC, 384], F32R, kind="ExternalInput").ap()
    ioa["cmask"] = nc.dram_tensor("cmask", [128, 4, QT], F32, kind="ExternalInput").ap()
    ioa["tri"] = nc.dram_tensor("tri", [128, 128], F32R, kind="ExternalInput").ap()
    ioa["ones"] = nc.dram_tensor("ones", [128, 128], F32R, kind="ExternalInput").ap()
    return ioa


def emit_attn(nc, s, A, ioa, o, S_len, NH, PS, tPS, o_dt, PSall=None):
    NT = S_len // QT
    NB = S_len // 128
    xT, wqkv, cmask, tri, ones = ioa["xT"], ioa["wqkv"], ioa["cmask"], ioa["tri"], ioa["ones"]
    W = A("W", [128, KC, 384], F32R)
    tW = T("W")
    XT = [A(f"XT{i}", [128, KC, QT], F32R) for i in range(2)]
    tXT = [T(f"XT{i}") for i in range(2)]
    KT = A("KT", [128, S_len], BF16)
    tKT = [T(f"KT{i}") for i in range(NT)]
    V = A("V", [128, NB, DH], BF16)
    tV = [T(f"V{i}") for i in range(NT)]
    Q = A("Q", [128, QT], BF16)
    tQ = T("Q")
    MASK = A("MASK", [128, 4, QT], F32)
    TRI = A("TRI", [128, 128], F32R)
    ONES = A("ONES", [128, 128], F32R)
    tC = T("consts")
    NBUF = 2
    E = [A(f"E{i}", [128, 2, QT], F32) for i in range(NBUF)]
    SP = [A(f"SP{i}", [128, 2, QT], F32R) for i in range(NBUF)]
    WT = [A(f"WT{i}", [128, 2, QT], F32) for i in range(NBUF)]
    AT = [A(f"AT{i}", [128, 2, QT], BF16) for i in range(NBUF)]
    tE = [T(f"E{i}") for i in range(NBUF)]
    tSP = [T(f"SP{i}") for i in range(NBUF)]
    tWT = [T(f"WT{i}") for i in range(NBUF)]
    tAT = [T(f"AT{i}") for i in range(NBUF)]
    U = [A(f"U{i}", [128, QT], F32R) for i in range(3)]
    tU = [T(f"U{i}") for i in range(3)]
    OS = [A(f"OS{i}", [128, QT], o_dt) for i in range(2)]
    tOS = [T(f"OS{i}") for i in range(2)]
    ZP = PSall[:, 0:2, :]
    GP = PSall[:, 2:4, :]
    tZP = [tPS[0], tPS[1]]
    tGP = [tPS[2], tPS[3]]
    ob = 4
    PP = (5, 6, 7)

    cd = s.dsem("const")
    s.dma("sp", MASK[:], cmask, writes=[tC], dsem=cd)
    s.dma("sp", TRI[:], tri, writes=[tC], dsem=cd)
    s.dma("sp", ONES[:], ones, writes=[tC], dsem=cd)

    pp_i = [0]

    def next_pp():
        b = PP[pp_i[0] % len(PP)]
        pp_i[0] += 1
        return b

    Q2 = [Q, A("Qb", [128, QT], BF16)]
    tQ2 = [tQ, T("Qb")]
    cnt = dict(x=0, o=0, p=0, u=0)
    scale = 1.0 / float(np.sqrt(DH))
    src = xT.rearrange("(kc p) t -> p kc t", p=128)
    CH = 4

    def proj_job(hl, i, load_w):
        if load_w:
            for c in range(4):
                s.dma("sp", W[:, c * 4:(c + 1) * 4, :], wqkv[hl, :, c * 4:(c + 1) * 4, :], writes=[tW])
        xb = cnt["x"] % 2
        cnt["x"] += 1
        qb = i % 2
        for c in range(4):
            s.dma("sp", XT[xb][:, c * 4:(c + 1) * 4, :], src[:, c * 4:(c + 1) * 4, i * QT:(i + 1) * QT], writes=[tXT[xb]])
        yield
        for which in range(2):
            b = next_pp()
            s.need("pe", reads=[tW, tXT[xb]], writes=[tPS[b]])
            for kc in range(KC):
                ins = nc.tensor.matmul(PS[b][:], W[:, kc, which * 128:(which + 1) * 128], XT[xb][:, kc, :], start=(kc == 0), stop=(kc == KC - 1))
                if kc % CH == CH - 1 and kc != KC - 1:
                    yield
            s.done("pe", ins, reads=[tW, tXT[xb]], writes=[tPS[b]])
            if which == 0:
                s.op("dve", lambda: nc.vector.tensor_scalar(Q2[qb][:], PS[b][:], scale, None, op0=ALU.mult),
                     reads=[tPS[b]], writes=[tQ2[qb]])
            else:
                s.op("dve", lambda: nc.vector.tensor_copy(KT[:, i * QT:(i + 1) * QT], PS[b][:]),
                     reads=[tPS[b]], writes=[tKT[i]])
            yield
        b = next_pp()
        s.need("pe", reads=[tW, tXT[xb]], writes=[tPS[b]])
        for tb in range(4):
            for kc in range(KC):
                ins = nc.tensor.matmul(PS[b][:, tb * 128:(tb + 1) * 128], XT[xb][:, kc, tb * 128:(tb + 1) * 128],
                                       W[:, kc, 256:384], start=(kc == 0), stop=(kc == KC - 1))
                if kc % CH == CH - 1 and not (tb == 3 and kc == KC - 1):
                    yield
        s.done("pe", ins, reads=[tW, tXT[xb]], writes=[tPS[b]])
        s.op("dve", lambda: nc.vector.tensor_copy(V[:, i * 4:(i + 1) * 4, :].rearrange("p a d -> p (a d)"), PS[b][:]),
             reads=[tPS[b]], writes=[tV[i]])

    def drain(job):
        if job is not None:
            for _ in job:
                pass

    def attention(hl, i, job):
        Qc, tQc = Q2[i % 2], tQ2[i % 2]
        npair = 2 * i + 2
        st = {}
        ucur = [None]
        pcnt = cnt["p"]

        def stageA(p):
            kb = 4 * i + 3 - 2 * p
            eb = (pcnt + p) % NBUF
            st[p] = eb
            s.need("pe", reads=[tKT[kb // 4], tQc], writes=tZP)
            nc.tensor.matmul(ZP[:, 0, :], KT[:, kb * 128:(kb + 1) * 128], Qc[:], start=True, stop=True)
            ins = nc.tensor.matmul(ZP[:, 1, :], KT[:, (kb - 1) * 128:kb * 128], Qc[:], start=True, stop=True)
            s.done("pe", ins, reads=[tKT[kb // 4], tQc], writes=tZP)
            s.op("act", lambda: nc.scalar.activation(out=E[eb][:], in_=ZP, func=AF.Exp), reads=tZP, writes=[tE[eb]])
            if p < 2:
                s.need("dve", reads=[tC, tE[eb]], writes=[tE[eb]])
                ins = nc.vector.tensor_tensor(E[eb][:], E[eb][:], MASK[:, 2 * p:2 * p + 2, :], op=ALU.mult)
                s.done("dve", ins, reads=[tC], writes=[tE[eb]])
            s.op("act", lambda: nc.scalar.activation(out=SP[eb][:], in_=E[eb][:], func=AF.Ln, bias=1.0),
                 reads=[tE[eb]], writes=[tSP[eb]])

        def stageB(p):
            eb = st[p]
            uc = ucur[0]
            rd = [tC, tSP[eb]] + ([tU[uc]] if uc is not None else [])
            s.need("pe", reads=rd, writes=tGP)
            nc.tensor.matmul(GP[:, 0, :], TRI[:], SP[eb][:, 0, :], start=True, stop=(uc is None))
            if uc is not None:
                nc.tensor.matmul(GP[:, 0, :], ONES[:], U[uc][:], start=False, stop=True)
            nc.tensor.matmul(GP[:, 1, :], TRI[:], SP[eb][:, 1, :], start=True, stop=False)
            ins = nc.tensor.matmul(GP[:, 1, :], ONES[:], SP[eb][:, 0, :], start=False, stop=(uc is None))
            if uc is not None:
                ins = nc.tensor.matmul(GP[:, 1, :], ONES[:], U[uc][:], start=False, stop=True)
            s.done("pe", ins, reads=rd, writes=tGP)
            if p < npair - 1:
                if uc is None:
                    u1 = cnt["u"] % 3
                    cnt["u"] += 1
                    s.op("pool", lambda: nc.gpsimd.tensor_tensor(U[u1][:], SP[eb][:, 0, :].bitcast(F32), SP[eb][:, 1, :].bitcast(F32), op=ALU.add),
                         reads=[tSP[eb]], writes=[tU[u1]])
                    ucur[0] = u1
                else:
                    u1 = cnt["u"] % 3
                    u2 = (cnt["u"] + 1) % 3
                    cnt["u"] += 2
                    s.op("pool", lambda: nc.gpsimd.tensor_tensor(U[u1][:], U[uc][:].bitcast(F32), SP[eb][:, 0, :].bitcast(F32), op=ALU.add),
                         reads=[tU[uc], tSP[eb]], writes=[tU[u1]])
                    s.op("dve", lambda: nc.vector.tensor_tensor(U[u2][:], U[u1][:].bitcast(F32), SP[eb][:, 1, :].bitcast(F32), op=ALU.add),
                         reads=[tU[u1], tSP[eb]], writes=[tU[u2]])
                    ucur[0] = u2
            s.op("act", lambda: nc.scalar.activation(out=WT[eb][:], in_=GP, func=AF.Exp, scale=-1.0),
                 reads=tGP, writes=[tWT[eb]])
            s.op("dve", lambda: nc.vector.tensor_tensor(AT[eb][:], E[eb][:], WT[eb][:], op=ALU.mult),
                 reads=[tE[eb], tWT[eb]], writes=[tAT[eb]])

        def stageC(p):
            kb = 4 * i + 3 - 2 * p
            eb = st[p]
            s.need("pe", reads=[tV[kb // 4], tAT[eb]], writes=[tPS[ob]] if p == 0 else [])
            nc.tensor.matmul(PS[ob][:], V[:, kb, :], AT[eb][:, 0, :], start=(p == 0), stop=False)
            ins = nc.tensor.matmul(PS[ob][:], V[:, kb - 1, :], AT[eb][:, 1, :], start=False, stop=(p == npair - 1))
            s.done("pe", ins, reads=[tV[kb // 4], tAT[eb]], writes=[tPS[ob]] if p == npair - 1 else [])

        for step in range(npair + 2):
            if step < npair:
                stageA(step)
            if 0 <= step - 1 < npair:
                stageB(step - 1)
                if job is not None:
                    next(job, None)
                    next(job, None)
            if 0 <= step - 2 < npair:
                stageC(step - 2)
        cnt["p"] += npair
        osb = cnt["o"] % 2
        cnt["o"] += 1
        s.op("dve", lambda: nc.vector.tensor_copy(OS[osb][:], PS[ob][:]), reads=[tPS[ob]], writes=[tOS[osb]])
        s.dma("sp", o[hl, :, i * QT:(i + 1) * QT], OS[osb][:], reads=[tOS[osb]])

    for hl in range(NH):
        drain(proj_job(hl, 0, True))
        for i in range(NT):
            job = proj_job(hl, i + 1, False) if i + 1 < NT else None
            if job is not None:
                next(job)
            attention(hl, i, job)
            drain(job)
    return tOS


def build_attn(S_len, NH=2):
    nc = bass.Bass("TRN2", target_bir_lowering=False)
    nc.dge_precook = False
    ioa = declare_attn_io(nc, S_len, NH)
    o = nc.dram_tensor("o", [NH, 128, S_len], F32, kind="ExternalOutput").ap()
    s = S(nc)
    PSt = nc.alloc_psum_tensor("psall", [128, 8, QT], F32)
    PSall = PSt[:]
    PS = [PSall[:, i, :] for i in range(8)]
    tPS = [T(f"ps{i}") for i in range(8)]
    A = lambda name, shape, dt: nc.alloc_sbuf_tensor(name, list(shape), dt)
    tOS = emit_attn(nc, s, A, ioa, o, S_len, NH, PS, tPS, F32, PSall=PSall)
    s.wait_all_dma("sp", [t.dsem for t in tOS if t.dsem is not None])
    return nc


def host_consts():
    sidx = np.arange(128)[:, None, None]
    j = np.array([3, 2, 1, 0])[None, :, None]
    t = np.arange(QT)[None, None, :]
    cmask = ((128 * j + sidx) < t).astype(np.float32)
    jj = np.arange(128)[:, None]
    ss = np.arange(128)[None, :]
    tri = (jj >= ss).astype(np.float32)
    ones = np.ones((128, 128), np.float32)
    return dict(cmask=np.ascontiguousarray(cmask), tri=tri, ones=ones)


D = 2048
KC = 16
TT = 512
NE = 8
ALPHA = float((2.0 * 2) ** 0.25)
EPS = 1e-5
POOL_W = (2, 4, 8, 16)
NSLAB = 5


def declare_main_io(nc, NTOK):
    io = {}

    def inp(name, shape, dt=F32R):
        io[name] = nc.dram_tensor(name, list(shape), dt, kind="ExternalInput").ap()

    inp("xTm", [D, NTOK])
    inp("xh", [128, KC, 16])
    inp("icnt", [128, 4, 16], F32)
    inp("e_win_p", [8, 128, KC, 128])
    inp("pool_w", [128, 16, 128])
    inp("pool_scale", [128, 8], F32)
    inp("e_wout", [16, 128, KC, 128])
    inp("ln_gb", [128, 10, KC], F32)
    inp("e_gu", [88, 128, KC, 128])
    inp("e_down", [64, 128, 11, 128])
    inp("o_win", [32, 128, KC, 128])
    inp("sgu_wT", [128, 8, 128], F32)
    inp("sgu_bb", [128, 8, 128], F32)
    inp("triu", [128, 128], F32)
    inp("o_wout", [16, 128, KC, 128])
    inp("router", [128, KC, 8], F32)
    inp("m_gu", [176, 128, KC, 128])
    inp("m_down", [128, 128, 11, 128])
    inp("ident", [128, 128], F32)
    inp("identr", [128, 128], F32R)
    inp("onesm", [128, 128], F32R)
    return io


_DS = {}


def _ds(s, name):
    k = (id(s), name)
    if k not in _DS:
        _DS[k] = s.dsem(name)
    return _DS[k]


def emit_main(nc, s, io, attnT, outT, NTOK, PS, tPS, A=None, gather=None):
    NTILE = NTOK // TT
    if A is None:
        A = lambda name, shape, dt: nc.alloc_sbuf_tensor(name, list(shape), dt)
    H = A("H", [128, KC, TT], F32R)
    tH = [T(f"H{k}") for k in range(KC)]
    Y = A("Y", [128, KC, TT], F32)
    tY = [T(f"Y{k}") for k in range(KC)]
    Yr = Y[:].bitcast(F32R)
    MIX = A("MIX", [128, KC, TT], F32R)
    tMIX = [T(f"MIX{k}") for k in range(KC)]
    SCR = A("SCR", [128, KC * TT], F32)
    tSCR = [T(f"SCR{k}") for k in range(KC)]
    SCRr = SCR[:].bitcast(F32R)
    SC3 = SCR[:].rearrange("p (c t) -> p c t", c=KC)
    SC3r = SCRr.rearrange("p (c t) -> p c t", c=KC)
    Yf = Y[:].rearrange("p c t -> p (c t)")
    PP = Yf[:, 0:8 * 528].rearrange("p (c t) -> p c t", c=8)
    TA = Yf[:, 9 * TT:9 * TT + 1056].rearrange("p (c t) -> p c t", c=2)
    TB = Yf[:, 12 * TT:12 * TT + 1056].rearrange("p (c t) -> p c t", c=2)
    tTA = tY[9:12]
    tTB = tY[12:15]
    DD = MIX[:, 8:16, :]
    UT = Y
    WSall = A("WS", [128, NSLAB, KC, 128], F32R)
    WS = [WSall[:, i] for i in range(NSLAB)]
    tWS = [T(f"WS{i}") for i in range(NSLAB)]
    XH = A("XH", [128, KC, 16], F32R)
    ICNT = A("ICNT", [128, 4, 16], F32)
    PSC = A("PSC", [128, 8], F32)
    LNGB = A("LNGB", [128, 10, KC], F32)
    SWTm = A("SWTm", [128, 8, 128], F32R)
    SBB = A("SBB", [128, 8, 128], F32)
    TRIU = A("TRIU", [128, 128], F32)
    RT = A("RT", [128, KC, 8], F32)
    IDENT = A("IDENT", [128, 128], F32)
    IDENTR = A("IDENTR", [128, 128], F32R)
    ONESM = A("ONESM", [128, 128], F32R)
    tC = T("constB")
    EPSB = A("EPSB", [128, 1], F32)
    PH = A("PH", [128, 8, 16], F32)
    tPH = T("PH")
    SQ = [A(f"SQ{i}", [128, TT], F32R) for i in range(2)]
    tSQ = [T(f"SQ{i}") for i in range(2)]
    MEAN = A("MEAN", [128, TT], F32)
    RSTD = A("RSTD", [128, TT], F32)
    tST = T("stats")
    NTMP = 3
    TMP = [A(f"TMP{i}", [128, TT], F32) for i in range(NTMP)]
    tTMP = [T(f"TMP{i}") for i in range(NTMP)]
    GBE = [A(f"GBE{i}", [128, TT], F32) for i in range(2)]
    tGBE = [T(f"GBE{i}") for i in range(2)]
    VTS = [A(f"VTS{i}", [128, TT], F32R) for i in range(2)]
    tVTS = [T(f"VTS{i}") for i in range(2)]
    SM = A("SM", [128, 64], F32)
    tSM = T("SM")
    LG = A("LG", [128, 4, 8], F32)
    GT = A("GT", [128, 4, 8], F32)
    M8 = A("M8", [128, 4, 8], F32)
    DG = [A(f"DG{i}", [128, 128], F32) for i in range(2)]
    tDG = [T(f"DG{i}") for i in range(2)]
    tLG = T("LG")

    SWT = Yf[:, 0:1024].rearrange("p (g t) -> p g t", g=8)
    cd = s.dsem("constB")
    for dst, src in ((XH[:], "xh"), (ICNT[:], "icnt"), (PSC[:], "pool_scale"), (LNGB[:], "ln_gb"),
                     (SWT, "sgu_wT"), (SBB[:], "sgu_bb"), (TRIU[:], "triu"), (RT[:], "router"),
                     (IDENT[:], "ident"), (IDENTR[:], "identr"), (ONESM[:], "onesm")):
        s.dma("sp", dst, io[src], writes=[tC] + tY[0:2], dsem=cd)
    s.op("pool", lambda: nc.gpsimd.memset(EPSB[:], EPS), reads=[], writes=[tC])
    for g in range(8):
        s.op("pool", lambda: nc.gpsimd.tensor_tensor(SWTm[:, g, :], SWT[:, g, :], TRIU[:], op=ALU.mult),
             reads=[tC] + tY[0:2], writes=[tC])

    st = dict(pb=0, ws=0, tmp=0, sq=0, dg=0, gbe=0, vts=0)

    def pp_tiles(ch):
        return tY[(ch * 2112) // 2048:((ch + 1) * 2112 - 1) // 2048 + 1]

    def bank():
        b = st["pb"] % 8
        st["pb"] += 1
        return b

    def load_slab(src_ap, nk=KC):
        i = st["ws"] % NSLAB
        st["ws"] += 1
        s.dma("sp", WS[i][:, 0:nk, :], src_ap, writes=[tWS[i]])
        return i

    def tmp():
        i = st["tmp"] % NTMP
        st["tmp"] += 1
        return i

    def mm_group(b, out_ap, pairs, reads):
        s.need("pe", reads=reads, writes=[tPS[b]])
        n = len(pairs)
        for i, (l, r) in enumerate(pairs):
            ins = nc.tensor.matmul(out_ap, l, r, start=(i == 0), stop=(i == n - 1))
        s.done("pe", ins, reads=reads, writes=[tPS[b]])

    def proj_chunk(w_ap, RHS, tRHS, nk=KC):
        sl = load_slab(w_ap, nk)
        b = bank()
        mm_group(b, PS[b][:], [(WS[sl][:, k, :], RHS[:, k, :]) for k in range(nk)], reads=[tWS[sl]] + list(tRHS[0:nk]))
        return b

    def layer_norm(gi, IN, INr, tIN, OUT, tOUT):
        b1 = bank()
        if INr is None:
            mm_group(b1, PS[b1][:], [(ONESM[:].bitcast(F32), IN[:, k, :]) for k in range(KC)], reads=list(tIN) + [tC])
        else:
            mm_group(b1, PS[b1][:], [(ONESM[:], INr[:, k, :]) for k in range(KC)], reads=list(tIN) + [tC])
        b2 = bank()
        s.need("pe", reads=[tC], writes=[tPS[b2]])
        for k in range(KC):
            q = st["sq"] % 2
            st["sq"] += 1
            s.op("act", lambda: nc.scalar.activation(out=SQ[q][:], in_=IN[:, k, :], func=AF.Square),
                 reads=[tIN[k]], writes=[tSQ[q]])
            s.op("pe", lambda: nc.tensor.matmul(PS[b2][:], ONESM[:], SQ[q][:], start=(k == 0), stop=(k == KC - 1)),
                 reads=[tSQ[q], tC], writes=[tPS[b2]] if k == KC - 1 else [])
        tm = tmp()
        s.op("dve", lambda: nc.vector.tensor_scalar(MEAN[:], PS[b1][:], 1.0 / D, None, op0=ALU.mult),
             reads=[tPS[b1]], writes=[tST])
        s.op("dve", lambda: nc.vector.tensor_tensor(TMP[tm][:], MEAN[:], MEAN[:], op=ALU.mult), reads=[tST], writes=[tTMP[tm]])
        s.op("dve", lambda: nc.vector.scalar_tensor_tensor(RSTD[:], PS[b2][:], 1.0 / D, TMP[tm][:], op0=ALU.mult, op1=ALU.subtract),
             reads=[tPS[b2], tTMP[tm]], writes=[tST])
        s.op("act", lambda: nc.scalar.activation(out=RSTD[:], in_=RSTD[:], func=AF.Sqrt, bias=EPSB[:]), reads=[tST, tC], writes=[tST])
        s.op("dve", lambda: nc.vector.reciprocal(RSTD[:], RSTD[:]), reads=[tST], writes=[tST])
        for k in range(KC):
            t1 = tmp()
            s.op("dve", lambda: nc.vector.tensor_tensor(TMP[t1][:], IN[:, k, :], MEAN[:], op=ALU.subtract),
                 reads=[tIN[k], tST], writes=[tTMP[t1]])
            s.op("pool", lambda: nc.gpsimd.tensor_tensor(TMP[t1][:], TMP[t1][:], RSTD[:], op=ALU.mult),
                 reads=[tST, tTMP[t1]], writes=[tTMP[t1]])
            s.op("act", lambda: nc.scalar.activation(out=OUT[:, k, :], in_=TMP[t1][:], func=AF.Identity,
                                                     scale=LNGB[:, gi, k:k + 1], bias=LNGB[:, gi + 1, k:k + 1]),
                 reads=[tTMP[t1], tC], writes=[tOUT[k]])

    def out_proj(w_name):
        for m in range(KC):
            b = proj_chunk(io[w_name][m], MIX, tMIX)
            s.op("dve", lambda: nc.vector.scalar_tensor_tensor(Y[:, m, :], H[:, m, :].bitcast(F32), ALPHA, PS[b][:],
                                                               op0=ALU.mult, op1=ALU.add),
                 reads=[tH[m], tPS[b]], writes=[tY[m]])

    def down_proj(w_name, slab0, first, final):
        for m in range(KC):
            b = proj_chunk(io[w_name][slab0 + m], SC3r, tSCR, nk=11)
            yo = Y[:, m, :]
            if first:
                s.op("dve", lambda: nc.vector.scalar_tensor_tensor(yo, H[:, m, :].bitcast(F32), ALPHA, PS[b][:],
                                                                   op0=ALU.mult, op1=ALU.add),
                     reads=[tH[m], tPS[b]], writes=[tY[m]])
            else:
                s.op("dve", lambda: nc.vector.tensor_tensor(yo, Y[:, m, :], PS[b][:], op=ALU.add),
                     reads=[tY[m], tPS[b]], writes=[tY[m]])

    def gu_chunk(w, idx, c, gate=None):
        bg = proj_chunk(w[2 * idx], H, tH)
        bu = proj_chunk(w[2 * idx + 1], H, tH)
        t1 = tmp()
        s.op("act", lambda: nc.scalar.activation(out=TMP[t1][:], in_=PS[bg][:], func=AF.Silu),
             reads=[tPS[bg]], writes=[tTMP[t1]])
        if gate is None:
            s.op("dve", lambda: nc.vector.tensor_tensor(SC3r[:, c, :], TMP[t1][:], PS[bu][:], op=ALU.mult),
                 reads=[tTMP[t1], tPS[bu]], writes=[tSCR[c]])
        else:
            s.op("dve", lambda: nc.vector.tensor_tensor(TMP[t1][:], TMP[t1][:], PS[bu][:], op=ALU.mult),
                 reads=[tTMP[t1], tPS[bu]], writes=[tTMP[t1]])
            s.op("pool", lambda: nc.gpsimd.tensor_tensor(SC3r[:, c, :], TMP[t1][:], GBE[gate][:], op=ALU.mult),
                 reads=[tTMP[t1], tGBE[gate]], writes=[tSCR[c]])

    xsrc = io["xTm"].rearrange("(kc p) t -> p kc t", p=128)
    if gather is None:
        asrc = attnT.rearrange("(kc p) t -> p kc t", p=128)
    else:
        gout, tG, selm = gather
        gv = gout.rearrange("(b r h p) (q t) -> p r h b q t", b=2, r=4, h=2, p=128, q=4)
    osrc = outT.rearrange("(kc p) t -> p kc t", p=128)

    for ti in range(NTILE):
        tsl = slice(ti * TT, (ti + 1) * TT)
        s.dma_multi("sp", [(H[:, c4 * 4:(c4 + 1) * 4, :], xsrc[:, c4 * 4:(c4 + 1) * 4, tsl]) for c4 in range(4)],
                    writes=tH, dsem=_ds(s, "H"))
        for ch in range(8):
            sl = load_slab(io["e_win_p"][ch])
            b = bank()
            mm_group(b, PS[b][:], [(WS[sl][:, k, :], H[:, k, :]) for k in range(KC)], reads=[tWS[sl]] + tH)
            s.op("act", lambda: nc.scalar.activation(out=PP[:, ch, 16:528], in_=PS[b][:], func=AF.Copy),
                 reads=[tPS[b]], writes=pp_tiles(ch))
            if ti == 0:
                b2 = bank()
                mm_group(b2, PS[b2][:, 0:16], [(WS[sl][:, k, :], XH[:, k, :]) for k in range(KC)], reads=[tWS[sl], tC])
                s.op("act", lambda: nc.scalar.activation(out=PP[:, ch, 0:16], in_=PS[b2][:, 0:16], func=AF.Copy),
                     reads=[tPS[b2]], writes=pp_tiles(ch))
        if ti > 0:
            s.op("pool", lambda: nc.gpsimd.tensor_copy(PP[:, :, 0:16], PH[:]), reads=[tPH], writes=tY[0:9])
        if ti < NTILE - 1:
            s.op("pool", lambda: nc.gpsimd.tensor_copy(PH[:], PP[:, :, 512:528]), reads=tY[0:9], writes=[tPH])
        for g, w in enumerate(POOL_W):
            Pg = PP[:, 2 * g:2 * g + 2, :]
            cur = Pg
            tcur = tY[0:9]
            sh = 1
            bufs = [(TA, tTA), (TB, tTB)]
            bi = 0
            lo = 0
            while sh < w:
                dst, tdst = bufs[bi % 2]
                bi += 1
                nlo = lo + sh
                eng = "pool" if (g + bi) % 2 == 0 else "dve"
                e = nc.gpsimd if eng == "pool" else nc.vector
                s.op(eng, lambda: e.tensor_tensor(dst[:, :, nlo:528], cur[:, :, nlo:528], cur[:, :, nlo - sh:528 - sh], op=ALU.add),
                     reads=list(tcur), writes=list(tdst))
                cur, tcur, lo = dst, tdst, nlo
                sh *= 2
            s.op("dve", lambda: nc.vector.scalar_tensor_tensor(DD[:, 2 * g:2 * g + 2, :], cur[:, :, 16:528], 1.0 / w, Pg[:, :, 16:528],
                                                               op0=ALU.mult, op1=ALU.subtract),
                 reads=list(tcur) + tY[0:9], writes=tMIX[8 + 2 * g:8 + 2 * g + 2])
            if ti == 0:
                for hh in range(2):
                    s.op("dve", lambda: nc.vector.tensor_tensor(TMP[0][:, 0:16], cur[:, hh, 16:32], ICNT[:, g, :], op=ALU.mult),
                         reads=list(tcur) + [tC], writes=[tTMP[0]])
                    s.op("dve", lambda: nc.vector.tensor_tensor(DD[:, 2 * g + hh, 0:16], TMP[0][:, 0:16], Pg[:, hh, 16:32], op=ALU.subtract),
                         reads=[tTMP[0]] + tY[0:9], writes=[tMIX[8 + 2 * g + hh]])
        slp = load_slab(io["pool_w"])
        for g in range(4):
            for mo in range(2):
                ch = 2 * g + mo
                b = bank()
                mm_group(b, PS[b][:], [(WS[slp][:, g * 4 + ki * 2 + mo, :], DD[:, 2 * g + ki, :]) for ki in range(2)],
                         reads=[tWS[slp], tMIX[8 + 2 * g], tMIX[8 + 2 * g + 1]])
                s.op("act", lambda: nc.scalar.activation(out=MIX[:, ch, :], in_=PS[b][:], func=AF.Identity, scale=PSC[:, ch:ch + 1]),
                     reads=[tPS[b], tC], writes=[tMIX[ch]])
        if gather is None:
            s.dma_multi("sp", [(MIX[:, 8 + c2 * 4:8 + (c2 + 1) * 4, :], asrc[:, c2 * 4:(c2 + 1) * 4, tsl]) for c2 in range(2)],
                        writes=tMIX[8:16], dsem=_ds(s, "MIXA"))
        else:
            s.dma_multi("sp", [(VTS[h][:].rearrange("p (j m) -> p j m", j=4), selm[:, h * 4:(h + 1) * 4, :]) for h in range(2)],
                        writes=tVTS, dsem=_ds(s, "SELM"))
            for c in range(8):
                hp, hl = divmod(c, 2)
                half = c % 2
                tcand = tSCR[half * 8:(half + 1) * 8]
                dst = SC3r[:, half * 8:(half + 1) * 8, :].rearrange("p (b q) t -> p b q t", b=2)
                s.dma_multi("sp", [(dst[:, bq], gv[:, hp, hl, bq, :, ti * TT:(ti + 1) * TT]) for bq in range(2)],
                            reads=[tG], writes=tcand, dsem=_ds(s, f"CAND{half}"))
                b = bank()
                mm_group(b, PS[b][:], [(VTS[j // 4][:, (j % 4) * 128:(j % 4 + 1) * 128], SC3r[:, half * 8 + j, :]) for j in range(8)],
                         reads=list(tcand) + tVTS)
                s.op("act", lambda: nc.scalar.activation(out=MIX[:, 8 + c, :], in_=PS[b][:], func=AF.Copy),
                     reads=[tPS[b]], writes=[tMIX[8 + c]])
        out_proj("e_wout")
        layer_norm(0, Y, None, tY, H, tH)
        for grp in range(4):
            for c in range(11):
                gu_chunk(io["e_gu"], grp * 11 + c, c)
            down_proj("e_down", grp * 16, first=(grp == 0), final=(grp == 3))
        layer_norm(2, Y, None, tY, H, tH)
        for m in range(KC):
            b = proj_chunk(io["o_win"][16 + m], H, tH)
            s.op("act", lambda: nc.scalar.activation(out=SC3r[:, m, :], in_=PS[b][:], func=AF.Gelu_apprx_tanh),
                 reads=[tPS[b]], writes=[tSCR[m]])
        for m in range(KC):
            b = proj_chunk(io["o_win"][m], H, tH)
            s.op("act", lambda: nc.scalar.activation(out=UT[:, m, :], in_=PS[b][:], func=AF.Gelu_apprx_tanh),
                 reads=[tPS[b]], writes=[tY[m]])
        layer_norm(8, SC3, SC3r, tSCR, SC3r, tSCR)
        for g in range(8):
            for hf in range(2):
                ch = 2 * g + hf
                b = bank()
                s.need("pe", reads=[tSCR[ch], tC], writes=[tPS[b]])
                for tb in range(4):
                    ins = nc.tensor.matmul(PS[b][:, tb * 128:(tb + 1) * 128], SC3r[:, ch, tb * 128:(tb + 1) * 128], IDENTR[:],
                                           start=True, stop=True)
                s.done("pe", ins, reads=[tSCR[ch], tC], writes=[tPS[b]])
                v = st["vts"] % 2
                st["vts"] += 1
                s.op("act", lambda: nc.scalar.activation(out=VTS[v][:], in_=PS[b][:], func=AF.Copy), reads=[tPS[b]], writes=[tVTS[v]])
                b2 = bank()
                s.need("pe", reads=[tVTS[v], tC], writes=[tPS[b2]])
                for tb in range(4):
                    ins = nc.tensor.matmul(PS[b2][:, tb * 128:(tb + 1) * 128], VTS[v][:, tb * 128:(tb + 1) * 128], SWTm[:, g, :],
                                           start=True, stop=True)
                s.done("pe", ins, reads=[tVTS[v], tC], writes=[tPS[b2]])
                t1 = tmp()
                s.need("dve", reads=[tPS[b2], tC], writes=[tTMP[t1]])
                for tb in range(4):
                    ins = nc.vector.tensor_tensor(TMP[t1][:, tb * 128:(tb + 1) * 128], PS[b2][:, tb * 128:(tb + 1) * 128],
                                                  SBB[:, g, :], op=ALU.add)
                s.done("dve", ins, reads=[tPS[b2], tC], writes=[tTMP[t1]])
                s.op("pool", lambda: nc.gpsimd.tensor_tensor(MIX[:, ch, :], TMP[t1][:], UT[:, ch, :], op=ALU.mult),
                     reads=[tTMP[t1], tY[ch]], writes=[tMIX[ch]])
        out_proj("o_wout")
        layer_norm(4, Y, None, tY, H, tH)
        o = 56
        for tb in range(4):
            b = bank()
            mm_group(b, PS[b][:, 0:8], [(H[:, k, tb * 128:(tb + 1) * 128].bitcast(F32), RT[:, k, :]) for k in range(KC)],
                     reads=tH + [tC])
            s.op("dve", lambda: nc.vector.tensor_copy(LG[:, tb, :], PS[b][:, 0:8]), reads=[tPS[b]], writes=[tLG])
            s.op("dve", lambda: nc.vector.max(out=M8[:, tb, :], in_=LG[:, tb, :]), reads=[tLG], writes=[tLG])
            s.op("dve", lambda: nc.vector.tensor_tensor(SM[:, o:o + 1], M8[:, tb, 1:2], M8[:, tb, 0:1], op=ALU.subtract),
                 reads=[tLG], writes=[tSM])
            s.op("act", lambda: nc.scalar.activation(out=SM[:, o + 1:o + 2], in_=SM[:, o:o + 1], func=AF.Exp), reads=[tSM], writes=[tSM])
            s.op("dve", lambda: nc.vector.tensor_scalar(SM[:, o + 2:o + 3], SM[:, o + 1:o + 2], 1.0, None, op0=ALU.add), reads=[tSM], writes=[tSM])
            s.op("dve", lambda: nc.vector.reciprocal(SM[:, o + 2:o + 3], SM[:, o + 2:o + 3]), reads=[tSM], writes=[tSM])
            s.op("dve", lambda: nc.vector.tensor_tensor(SM[:, o + 3:o + 4], SM[:, o + 1:o + 2], SM[:, o + 2:o + 3], op=ALU.mult),
                 reads=[tSM], writes=[tSM])
            s.op("dve", lambda: nc.vector.tensor_scalar(GT[:, tb, :], LG[:, tb, :], M8[:, tb, 0:1], SM[:, o + 2:o + 3],
                                                        op0=ALU.is_equal, op1=ALU.mult), reads=[tLG, tSM], writes=[tLG])
            s.op("dve", lambda: nc.vector.tensor_scalar(LG[:, tb, :], LG[:, tb, :], M8[:, tb, 1:2], SM[:, o + 3:o + 4],
                                                        op0=ALU.is_equal, op1=ALU.mult), reads=[tLG, tSM], writes=[tLG])
            s.op("dve", lambda: nc.vector.tensor_tensor(GT[:, tb, :], GT[:, tb, :], LG[:, tb, :], op=ALU.add), reads=[tLG], writes=[tLG])
        for e in range(NE):
            b = bank()
            s.need("pe", reads=[tC], writes=[tPS[b]])
            for tb in range(4):
                q = st["dg"] % 2
                st["dg"] += 1
                s.op("dve", lambda: nc.vector.tensor_scalar(DG[q][:], IDENT[:], GT[:, tb, e:e + 1], None, op0=ALU.mult),
                     reads=[tC, tLG], writes=[tDG[q]])
                s.op("pe", lambda: nc.tensor.matmul(PS[b][:, tb * 128:(tb + 1) * 128], ONESM[:].bitcast(F32), DG[q][:], start=True, stop=True),
                     reads=[tDG[q], tC], writes=[tPS[b]] if tb == 3 else [])
            ge = st["gbe"] % 2
            st["gbe"] += 1
            s.op("act", lambda: nc.scalar.activation(out=GBE[ge][:], in_=PS[b][:], func=AF.Copy), reads=[tPS[b]], writes=[tGBE[ge]])
            for c in range(11):
                gu_chunk(io["m_gu"], e * 11 + c, c, gate=ge)
            down_proj("m_down", e * 16, first=(e == 0), final=(e == NE - 1))
        layer_norm(6, Y, None, tY, MIX, tMIX)
        s.dma_multi("sp", [(osrc[:, c4 * 4:(c4 + 1) * 4, tsl], MIX[:, c4 * 4:(c4 + 1) * 4, :].bitcast(F32)) for c4 in range(4)],
                    reads=tMIX, dsem=_ds(s, "OUT"))
    s.wait_all_dma("sp", [_ds(s, "OUT")])


def build_fused(S_len, NTOK):
    nc = bass.Bass("TRN2", target_bir_lowering=False)
    nc.dge_precook = False
    ioa = declare_attn_io(nc, S_len, 2)
    io = declare_main_io(nc, NTOK)
    selm = nc.dram_tensor("selm", [128, 8, 128], F32R, kind="ExternalInput").ap()
    outT = nc.dram_tensor("outT", [D, NTOK], F32, kind="ExternalOutput").ap()
    gin = nc.dram_tensor("gin", [256, S_len], F32R)
    gout = nc.dram_tensor("gout", [8 * 256, S_len], F32R)
    s = S(nc)
    PSt = nc.alloc_psum_tensor("psall", [128, 8, 512], F32)
    PSall = PSt[:]
    PS = [PSall[:, i, :] for i in range(8)]
    tPS = [T(f"ps{i}") for i in range(8)]
    with ExitStack() as stack:
        A1 = lambda name, shape, dt: stack.enter_context(nc.sbuf_tensor(name, list(shape), dt))
        tOS = emit_attn(nc, s, A1, ioa, gin.ap().rearrange("(h p) t -> h p t", p=128), S_len, 2, PS, tPS, F32R, PSall=PSall)
        s.need("pool", writes=tOS)
        ccsem = nc.alloc_semaphore("cc")
        s.semobj["cc"] = ccsem
        nc.gpsimd.collective_compute("AllGather", ALU.bypass, replica_groups=[list(range(8))],
                                     ins=[gin.ap()], outs=[gout.ap()]).then_inc(ccsem, 1)
        tG = T("G")
        tG.w = ("cc", 1)
        s.snap[("cc", 1)] = dict(s.known["pool"])
        s.barrier()
    A2 = lambda name, shape, dt: nc.alloc_sbuf_tensor(name, list(shape), dt)
    emit_main(nc, s, io, None, outT, NTOK, PS, tPS, A=A2, gather=(gout.ap(), tG, selm))
    print("instructions", s.ninst, "waits", s.nwait)
    return nc


def host_selm(b, tq):
    m = np.zeros((128, 8, 128), np.float32)
    m[:, b * 4 + tq, :] = np.eye(128, dtype=np.float32)
    return m


POOL_W = (2, 4, 8, 16)

def tile_kn(W):
    K, N = W.shape
    return W.reshape(K // 128, 128, N // 128, 128).transpose(2, 1, 0, 3)

def vec_pk(v):
    return v.reshape(-1, 128).T

def host_weights(p):
    f = np.float32
    o = {}
    o["e_win_p"] = tile_kn(p["even_w_in"][0][:, :1024])
    o["pool_w"] = p["even_pool_w"][0].reshape(4, 2, 128, 2, 128).transpose(2, 0, 1, 3, 4).reshape(128, 16, 128)
    o["pool_scale"] = p["even_pool_scale"][0].reshape(8, 128).T
    o["e_wout"] = tile_kn(p["even_w_out"][0])
    vs = [p["even_ln1_g"][0], p["even_ln1_b"][0], p["even_ln2_g"][0], p["even_ln2_b"][0],
          p["odd_ln1_g"][0], p["odd_ln1_b"][0], p["odd_ln2_g"][0], p["odd_ln2_b"][0],
          p["odd_sgu_ln_g"][0], p["odd_sgu_ln_b"][0]]
    o["ln_gb"] = np.stack([vec_pk(v) for v in vs], axis=1)
    gu = p["even_ffn_w_gu"][0]
    o["e_gu"] = np.stack([tile_kn(gu[:, :5632]), tile_kn(gu[:, 5632:])], axis=1).reshape(88, 128, 16, 128)
    wd = p["even_ffn_w_down"][0]
    o["e_down"] = wd.reshape(4, 11, 128, 16, 128).transpose(0, 3, 2, 1, 4).reshape(64, 128, 11, 128)
    o["o_win"] = tile_kn(p["odd_w_in"][0])
    o["sgu_wT"] = p["odd_sgu_w"][0].transpose(2, 0, 1)
    o["sgu_bb"] = np.broadcast_to(p["odd_sgu_b"][0][None], (128, 8, 128))
    o["triu"] = np.triu(np.ones((128, 128), f))
    o["o_wout"] = tile_kn(p["odd_w_out"][0])
    o["router"] = p["odd_router"][0].reshape(16, 128, 8).transpose(1, 0, 2)
    mg = p["odd_moe_w_gu"][0]
    o["m_gu"] = np.stack([np.stack([tile_kn(mg[e][:, :1408]), tile_kn(mg[e][:, 1408:])], axis=1) for e in range(8)], axis=0).reshape(176, 128, 16, 128)
    md = p["odd_moe_w_down"][0]
    o["m_down"] = md.reshape(8, 11, 128, 16, 128).transpose(0, 3, 2, 1, 4).reshape(128, 128, 11, 128)
    o["ident"] = np.eye(128, dtype=f)
    o["identr"] = np.eye(128, dtype=f)
    o["onesm"] = np.ones((128, 128), f)
    return {k: np.ascontiguousarray(v, dtype=f) for k, v in o.items()}

def host_icnt(at_start):
    ic = np.zeros((128, 4, 16), np.float32)
    for g, w in enumerate(POOL_W):
        if at_start:
            ic[:, g, :] = 1.0 / np.minimum(np.arange(16) + 1, w)
        else:
            ic[:, g, :] = 1.0 / w
    return ic

def host_xh(xprev):
    return np.ascontiguousarray(xprev.T.reshape(16, 128, 16).transpose(1, 0, 2), dtype=np.float32)


SEQ = 16384
BATCH = 2
NTOKC = 4096


def kernel(**inputs):
    p = {k: np.asarray(v, dtype=np.float32) for k, v in inputs.items()}
    x = p["x"]
    f = np.float32
    xT = [np.ascontiguousarray(x[b].T) for b in range(BATCH)]
    w_in = p["even_w_in"][0]
    consts = host_consts()
    hw = host_weights(p)
    in_maps = []
    for c in range(8):
        b, j = divmod(c, 4)
        ws = []
        for hl in range(2):
            h = 2 * j + hl
            wcat = np.concatenate([w_in[:, 1024 + h * 128:1024 + (h + 1) * 128],
                                   w_in[:, 2048 + h * 128:2048 + (h + 1) * 128],
                                   w_in[:, 3072 + h * 128:3072 + (h + 1) * 128]], axis=1)
            ws.append(wcat.reshape(KC, 128, 384).transpose(1, 0, 2))
        t0 = j * NTOKC
        m = dict(hw)
        m.update(consts)
        m["xT"] = xT[b]
        m["wqkv"] = np.ascontiguousarray(np.stack(ws, 0), dtype=f)
        m["xTm"] = np.ascontiguousarray(xT[b][:, t0:t0 + NTOKC])
        m["xh"] = host_xh(x[b, t0 - 16:t0] if j > 0 else np.zeros((16, D), f))
        m["icnt"] = host_icnt(j == 0)
        m["selm"] = host_selm(b, j)
        in_maps.append(m)
    nc = build_fused(SEQ, NTOKC)
    res = run_bass_kernel_spmd(nc, in_maps, core_ids=list(range(8)))
    out = np.empty((BATCH, SEQ, D), f)
    for c in range(8):
        b, j = divmod(c, 4)
        out[b, j * NTOKC:(j + 1) * NTOKC, :] = np.asarray(res.results[c]["outT"]).T
    return out
```

```python
from contextlib import ExitStack
import numpy as np
import concourse.bass as bass
import concourse.mybir as mybir
from concourse.bass_utils import run_bass_kernel_spmd


F32 = mybir.dt.float32
F32R = mybir.dt.float32r
BF16 = mybir.dt.bfloat16
AF = mybir.ActivationFunctionType
ALU = mybir.AluOpType


class T:
    __slots__ = ("name", "w", "r", "dsem")

    def __init__(self, name=""):
        self.name = name
        self.w = None
        self.r = {}
        self.dsem = None


class DSem:
    def __init__(self, S, name):
        self.key = ("d", name)
        self.sem = S.nc.alloc_semaphore("d_" + name)
        self.val = 0
        S.semobj[self.key] = self.sem
        S.dsems.append(self)


class S:
    ENG = ("pe", "act", "dve", "pool", "sp")

    def __init__(self, nc, same_engine_sync=True):
        self.nc = nc
        self.eng = {"pe": nc.tensor, "act": nc.scalar, "dve": nc.vector, "pool": nc.gpsimd, "sp": nc.sync}
        self.semobj = {}
        self.cnt = {}
        self.known = {}
        for e in self.ENG:
            self.semobj[e] = nc.alloc_semaphore("s_" + e)
            self.cnt[e] = 0
            self.known[e] = {}
        self.snap = {}
        self.dsems = []
        self.same = same_engine_sync
        self.nwait = 0
        self.ninst = 0
        self._dn = 0

    def dsem(self, name=None):
        self._dn += 1
        return DSem(self, name or f"x{self._dn}")

    def need(self, e, reads=(), writes=()):
        req = {}
        for t in reads:
            if t.w is not None:
                k, v = t.w
                if req.get(k, 0) < v:
                    req[k] = v
        for t in writes:
            if t.w is not None:
                k, v = t.w
                if req.get(k, 0) < v:
                    req[k] = v
            for k, v in t.r.items():
                if req.get(k, 0) < v:
                    req[k] = v
        kn = self.known[e]
        for k, v in req.items():
            if kn.get(k, 0) >= v:
                continue
            if k == e and not self.same:
                continue
            self.eng[e].wait_ge(self.semobj[k], v)
            self.nwait += 1
            if kn.get(k, 0) < v:
                kn[k] = v
            sn = self.snap.get((k, v))
            if sn:
                for k2, v2 in sn.items():
                    if kn.get(k2, 0) < v2:
                        kn[k2] = v2

    def done(self, e, ins, reads=(), writes=()):
        self.cnt[e] += 1
        ins.then_inc(self.semobj[e], 1)
        ev = (e, self.cnt[e])
        self.snap[ev] = dict(self.known[e])
        for t in reads:
            if t.r.get(e, 0) < ev[1]:
                t.r[e] = ev[1]
        for t in writes:
            t.w = ev
            t.r = {}
        self.ninst += 1
        return ev

    def op(self, e, fn, reads=(), writes=()):
        self.need(e, reads, writes)
        ins = fn()
        return self.done(e, ins, reads, writes)

    def dma(self, q, out_ap, in_ap, reads=(), writes=(), dsem=None):
        self.need(q, reads, writes)
        if dsem is None:
            t = (list(writes) + list(reads))[0]
            if t.dsem is None:
                t.dsem = self.dsem(t.name or None)
            dsem = t.dsem
        dsem.val += 16
        self.eng[q].dma_start(out=out_ap, in_=in_ap).then_inc(dsem.sem, 16)
        ev = (dsem.key, dsem.val)
        self.snap[ev] = dict(self.known[q])
        for t in reads:
            t.r[dsem.key] = dsem.val
        for t in writes:
            t.w = ev
            t.r = {}
        self.ninst += 1
        return ev

    def dma_multi(self, q, pairs, reads=(), writes=(), dsem=None):
        self.need(q, reads, writes)
        for out_ap, in_ap in pairs:
            dsem.val += 16
            self.eng[q].dma_start(out=out_ap, in_=in_ap).then_inc(dsem.sem, 16)
            self.ninst += 1
        ev = (dsem.key, dsem.val)
        self.snap[ev] = dict(self.known[q])
        for t in reads:
            t.r[dsem.key] = dsem.val
        for t in writes:
            t.w = ev
            t.r = {}
        return ev

    def barrier(self):
        keys = {e: self.cnt[e] for e in self.ENG}
        for d in self.dsems:
            keys[d.key] = d.val
        for e in self.ENG:
            kn = self.known[e]
            for k, v in keys.items():
                if v > 0 and kn.get(k, 0) < v:
                    self.eng[e].wait_ge(self.semobj[k], v)
                    self.nwait += 1
                    kn[k] = v

    def wait_all_dma(self, q, dsems):
        for d in dsems:
            if d.val > 0:
                self.eng[q].wait_ge(d.sem, d.val)


DH = 128
QT = 512


def declare_attn_io(nc, S_len, NH=2):
    ioa = {}
    ioa["xT"] = nc.dram_tensor("xT", [D, S_len], F32R, kind="ExternalInput").ap()
    ioa["wqkv"] = nc.dram_tensor("wqkv", [NH, 128, KC, 384], F32R, kind="ExternalInput").ap()
    ioa["cmask"] = nc.dram_tensor("cmask", [128, 4, QT], F32, kind="ExternalInput").ap()
    ioa["tri"] = nc.dram_tensor("tri", [128, 128], F32R, kind="ExternalInput").ap()
    ioa["ones"] = nc.dram_tensor("ones", [128, 128], F32R, kind="ExternalInput").ap()
    return ioa


def emit_attn(nc, s, A, ioa, o, S_len, NH, PS, tPS, o_dt, PSall=None, after_head=None):
    NT = S_len // QT
    NB = S_len // 128
    xT, wqkv, cmask, tri, ones = ioa["xT"], ioa["wqkv"], ioa["cmask"], ioa["tri"], ioa["ones"]
    W = A("W", [128, KC, 384], F32R)
    tW = T("W")
    XT = [A(f"XT{i}", [128, KC, QT], F32R) for i in range(2)]
    tXT = [T(f"XT{i}") for i in range(2)]
    KT = A("KT", [128, S_len], BF16)
    tKT = [T(f"KT{i}") for i in range(NT)]
    V = A("V", [128, NB, DH], BF16)
    tV = [T(f"V{i}") for i in range(NT)]
    Q = A("Q", [128, QT], BF16)
    tQ = T("Q")
    MASK = A("MASK", [128, 4, QT], F32)
    TRI = A("TRI", [128, 128], F32R)
    ONES = A("ONES", [128, 128], F32R)
    tC = T("consts")
    NBUF = 2
    NEB = 3
    E = [A(f"E{i}", [128, 2, QT], F32) for i in range(NEB)]
    SP = [A(f"SP{i}", [128, 2, QT], F32R) for i in range(NBUF)]
    WT = [A(f"WT{i}", [128, 2, QT], F32) for i in range(NBUF)]
    NAT = 3
    AT = [A(f"AT{i}", [128, 2, QT], BF16) for i in range(NAT)]
    tE = [T(f"E{i}") for i in range(NEB)]
    tSP = [T(f"SP{i}") for i in range(NBUF)]
    tWT = [T(f"WT{i}") for i in range(NBUF)]
    tAT = [T(f"AT{i}") for i in range(NAT)]
    U = [A(f"U{i}", [128, QT], F32R) for i in range(3)]
    tU = [T(f"U{i}") for i in range(3)]
    OS = [A(f"OS{i}", [128, QT], o_dt) for i in range(2)]
    tOS = [T(f"OS{i}") for i in range(2)]
    ZP = PSall[:, 0:2, :]
    GP = PSall[:, 2:4, :]
    tZP = [tPS[0], tPS[1]]
    tGP = [tPS[2], tPS[3]]
    ob = 4
    PP = (5, 6, 7)

    cd = s.dsem("const")
    s.dma("sp", MASK[:], cmask, writes=[tC], dsem=cd)
    s.dma("sp", TRI[:], tri, writes=[tC], dsem=cd)
    s.dma("sp", ONES[:], ones, writes=[tC], dsem=cd)

    pp_i = [0]

    def next_pp():
        b = PP[pp_i[0] % len(PP)]
        pp_i[0] += 1
        return b

    Q2 = [Q, A("Qb", [128, QT], BF16)]
    tQ2 = [tQ, T("Qb")]
    cnt = dict(x=0, o=0, p=0, u=0)
    scale = 1.0 / float(np.sqrt(DH))
    src = xT.rearrange("(kc p) t -> p kc t", p=128)
    CH = 4

    def proj_job(hl, i, load_w):
        if load_w:
            for c in range(4):
                s.dma("sp", W[:, c * 4:(c + 1) * 4, :], wqkv[hl, :, c * 4:(c + 1) * 4, :], writes=[tW])
        xb = cnt["x"] % 2
        cnt["x"] += 1
        qb = i % 2
        for c in range(4):
            s.dma("sp", XT[xb][:, c * 4:(c + 1) * 4, :], src[:, c * 4:(c + 1) * 4, i * QT:(i + 1) * QT], writes=[tXT[xb]])
        yield
        for which in range(2):
            b = next_pp()
            s.need("pe", reads=[tW, tXT[xb]], writes=[tPS[b]])
            for kc in range(KC):
                ins = nc.tensor.matmul(PS[b][:], W[:, kc, which * 128:(which + 1) * 128], XT[xb][:, kc, :], start=(kc == 0), stop=(kc == KC - 1))
                if kc % CH == CH - 1 and kc != KC - 1:
                    yield
            s.done("pe", ins, reads=[tW, tXT[xb]], writes=[tPS[b]])
            if which == 0:
                s.op("dve", lambda: nc.vector.tensor_scalar(Q2[qb][:], PS[b][:], scale, None, op0=ALU.mult),
                     reads=[tPS[b]], writes=[tQ2[qb]])
            else:
                s.op("dve", lambda: nc.vector.tensor_copy(KT[:, i * QT:(i + 1) * QT], PS[b][:]),
                     reads=[tPS[b]], writes=[tKT[i]])
            yield
        b = next_pp()
        s.need("pe", reads=[tW, tXT[xb]], writes=[tPS[b]])
        for tb in range(4):
            for kc in range(KC):
                ins = nc.tensor.matmul(PS[b][:, tb * 128:(tb + 1) * 128], XT[xb][:, kc, tb * 128:(tb + 1) * 128],
                                       W[:, kc, 256:384], start=(kc == 0), stop=(kc == KC - 1))
                if kc % CH == CH - 1 and not (tb == 3 and kc == KC - 1):
                    yield
        s.done("pe", ins, reads=[tW, tXT[xb]], writes=[tPS[b]])
        s.op("dve", lambda: nc.vector.tensor_copy(V[:, i * 4:(i + 1) * 4, :].rearrange("p a d -> p (a d)"), PS[b][:]),
             reads=[tPS[b]], writes=[tV[i]])

    def drain(job):
        if job is not None:
            for _ in job:
                pass

    def attention(hl, i, job):
        Qc, tQc = Q2[i % 2], tQ2[i % 2]
        npair = 2 * i + 2
        st = {}
        ucur = [None]
        pcnt = cnt["p"]

        def stageA(p):
            kb = 4 * i + 3 - 2 * p
            eb = (pcnt + p) % NBUF
            ee = (pcnt + p) % NEB
            st[p] = eb
            s.need("pe", reads=[tKT[kb // 4], tQc], writes=tZP)
            nc.tensor.matmul(ZP[:, 0, :], KT[:, kb * 128:(kb + 1) * 128], Qc[:], start=True, stop=True)
            ins = nc.tensor.matmul(ZP[:, 1, :], KT[:, (kb - 1) * 128:kb * 128], Qc[:], start=True, stop=True)
            s.done("pe", ins, reads=[tKT[kb // 4], tQc], writes=tZP)
            s.op("act", lambda: nc.scalar.activation(out=E[ee][:], in_=ZP, func=AF.Exp), reads=tZP, writes=[tE[ee]])
            if p < 2:
                s.need("dve", reads=[tC, tE[ee]], writes=[tE[ee]])
                ins = nc.vector.tensor_tensor(E[ee][:], E[ee][:], MASK[:, 2 * p:2 * p + 2, :], op=ALU.mult)
                s.done("dve", ins, reads=[tC], writes=[tE[ee]])

        def stageA2(p):
            eb = st[p]
            ee = (pcnt + p) % NEB
            s.op("act", lambda: nc.scalar.activation(out=SP[eb][:], in_=E[ee][:], func=AF.Ln, bias=1.0),
                 reads=[tE[ee]], writes=[tSP[eb]])

        def stageB(p):
            eb = st[p]
            uc = ucur[0]
            rd = [tC, tSP[eb]] + ([tU[uc]] if uc is not None else [])
            s.need("pe", reads=rd, writes=tGP)
            nc.tensor.matmul(GP[:, 0, :], TRI[:], SP[eb][:, 0, :], start=True, stop=(uc is None))
            if uc is not None:
                nc.tensor.matmul(GP[:, 0, :], ONES[:], U[uc][:], start=False, stop=True)
            nc.tensor.matmul(GP[:, 1, :], TRI[:], SP[eb][:, 1, :], start=True, stop=False)
            ins = nc.tensor.matmul(GP[:, 1, :], ONES[:], SP[eb][:, 0, :], start=False, stop=(uc is None))
            if uc is not None:
                ins = nc.tensor.matmul(GP[:, 1, :], ONES[:], U[uc][:], start=False, stop=True)
            s.done("pe", ins, reads=rd, writes=tGP)
            if p < npair - 1:
                if uc is None:
                    u1 = cnt["u"] % 3
                    cnt["u"] += 1
                    s.op("pool", lambda: nc.gpsimd.tensor_tensor(U[u1][:], SP[eb][:, 0, :].bitcast(F32), SP[eb][:, 1, :].bitcast(F32), op=ALU.add),
                         reads=[tSP[eb]], writes=[tU[u1]])
                    ucur[0] = u1
                else:
                    u1 = cnt["u"] % 3
                    u2 = (cnt["u"] + 1) % 3
                    cnt["u"] += 2
                    s.op("pool", lambda: nc.gpsimd.tensor_tensor(U[u1][:], U[uc][:].bitcast(F32), SP[eb][:, 0, :].bitcast(F32), op=ALU.add),
                         reads=[tU[uc], tSP[eb]], writes=[tU[u1]])
                    s.op("dve", lambda: nc.vector.tensor_tensor(U[u2][:], U[u1][:].bitcast(F32), SP[eb][:, 1, :].bitcast(F32), op=ALU.add),
                         reads=[tU[u1], tSP[eb]], writes=[tU[u2]])
                    ucur[0] = u2
            s.op("act", lambda: nc.scalar.activation(out=WT[eb][:], in_=GP, func=AF.Exp, scale=-1.0),
                 reads=tGP, writes=[tWT[eb]])

        def stageB2(p):
            eb = st[p]
            ab = (pcnt + p) % NAT
            ee = (pcnt + p) % NEB
            s.op("dve", lambda: nc.vector.tensor_tensor(AT[ab][:], E[ee][:], WT[eb][:], op=ALU.mult),
                 reads=[tE[ee], tWT[eb]], writes=[tAT[ab]])

        def stageC(p):
            kb = 4 * i + 3 - 2 * p
            ab = (pcnt + p) % NAT
            s.need("pe", reads=[tV[kb // 4], tAT[ab]], writes=[tPS[ob]] if p == 0 else [])
            nc.tensor.matmul(PS[ob][:], V[:, kb, :], AT[ab][:, 0, :], start=(p == 0), stop=False)
            ins = nc.tensor.matmul(PS[ob][:], V[:, kb - 1, :], AT[ab][:, 1, :], start=False, stop=(p == npair - 1))
            s.done("pe", ins, reads=[tV[kb // 4], tAT[ab]], writes=[tPS[ob]] if p == npair - 1 else [])

        for step in range(npair + 2):
            if step < npair:
                stageA(step)
                stageA2(step)
            if 0 <= step - 1 < npair:
                stageB(step - 1)
                stageB2(step - 1)
                if job is not None:
                    next(job, None)
                    next(job, None)
            if 0 <= step - 2 < npair:
                stageC(step - 2)
        cnt["p"] += npair
        osb = cnt["o"] % 2
        cnt["o"] += 1
        s.op("dve", lambda: nc.vector.tensor_copy(OS[osb][:], PS[ob][:]), reads=[tPS[ob]], writes=[tOS[osb]])
        s.dma("sp", o[hl, :, i * QT:(i + 1) * QT], OS[osb][:], reads=[tOS[osb]])

    for hl in range(NH):
        drain(proj_job(hl, 0, True))
        for i in range(NT):
            job = proj_job(hl, i + 1, False) if i + 1 < NT else None
            if job is not None:
                next(job)
            attention(hl, i, job)
            drain(job)
        if after_head is not None:
            after_head(hl, tOS)
    return tOS


def build_attn(S_len, NH=2):
    nc = bass.Bass("TRN2", target_bir_lowering=False)
    nc.dge_precook = False
    ioa = declare_attn_io(nc, S_len, NH)
    o = nc.dram_tensor("o", [NH, 128, S_len], F32, kind="ExternalOutput").ap()
    s = S(nc)
    PSt = nc.alloc_psum_tensor("psall", [128, 8, QT], F32)
    PSall = PSt[:]
    PS = [PSall[:, i, :] for i in range(8)]
    tPS = [T(f"ps{i}") for i in range(8)]
    A = lambda name, shape, dt: nc.alloc_sbuf_tensor(name, list(shape), dt)
    tOS = emit_attn(nc, s, A, ioa, o, S_len, NH, PS, tPS, F32, PSall=PSall)
    s.wait_all_dma("sp", [t.dsem for t in tOS if t.dsem is not None])
    return nc


def host_consts():
    sidx = np.arange(128)[:, None, None]
    j = np.array([3, 2, 1, 0])[None, :, None]
    t = np.arange(QT)[None, None, :]
    cmask = ((128 * j + sidx) < t).astype(np.float32)
    jj = np.arange(128)[:, None]
    ss = np.arange(128)[None, :]
    tri = (jj >= ss).astype(np.float32)
    ones = np.ones((128, 128), np.float32)
    return dict(cmask=np.ascontiguousarray(cmask), tri=tri, ones=ones)


D = 2048
KC = 16
TT = 512
NE = 8
ALPHA = float((2.0 * 2) ** 0.25)
EPS = 1e-5
POOL_W = (2, 4, 8, 16)
NSLAB = 5


def declare_main_io(nc, NTOK):
    io = {}

    def inp(name, shape, dt=F32R):
        io[name] = nc.dram_tensor(name, list(shape), dt, kind="ExternalInput").ap()

    inp("xTm", [D, NTOK])
    inp("xh", [128, KC, 16])
    inp("icnt", [128, 4, 16], F32)
    inp("e_win_p", [8, 128, KC, 128])
    inp("pool_w", [128, 16, 128])
    inp("pool_scale", [128, 8], F32)
    inp("e_wout", [16, 128, KC, 128])
    inp("ln_gb", [128, 10, KC], F32)
    inp("e_gu", [88, 128, KC, 128])
    inp("e_down", [64, 128, 11, 128])
    inp("o_win", [32, 128, KC, 128])
    inp("sgu_wT", [128, 8, 128], F32)
    inp("sgu_bb", [128, 8, 128], F32)
    inp("triu", [128, 128], F32)
    inp("o_wout", [16, 128, KC, 128])
    inp("router", [128, KC, 8], F32)
    inp("m_gu", [176, 128, KC, 128])
    inp("m_down", [128, 128, 11, 128])
    inp("ident", [128, 128], F32)
    inp("identr", [128, 128], F32R)
    inp("onesm", [128, 128], F32R)
    return io


_DS = {}


def _ds(s, name):
    k = (id(s), name)
    if k not in _DS:
        _DS[k] = s.dsem(name)
    return _DS[k]


def emit_main(nc, s, io, attnT, outT, NTOK, PS, tPS, A=None, gather=None):
    NTILE = NTOK // TT
    if A is None:
        A = lambda name, shape, dt: nc.alloc_sbuf_tensor(name, list(shape), dt)
    H = A("H", [128, KC, TT], F32R)
    tH = [T(f"H{k}") for k in range(KC)]
    Y = A("Y", [128, KC, TT], F32)
    tY = [T(f"Y{k}") for k in range(KC)]
    Yr = Y[:].bitcast(F32R)
    MIX = A("MIX", [128, KC, TT], F32R)
    tMIX = [T(f"MIX{k}") for k in range(KC)]
    SCR = A("SCR", [128, KC * TT], F32)
    tSCR = [T(f"SCR{k}") for k in range(KC)]
    SCRr = SCR[:].bitcast(F32R)
    SC3 = SCR[:].rearrange("p (c t) -> p c t", c=KC)
    SC3r = SCRr.rearrange("p (c t) -> p c t", c=KC)
    Yf = Y[:].rearrange("p c t -> p (c t)")
    PP = Yf[:, 0:8 * 528].rearrange("p (c t) -> p c t", c=8)
    TA = Yf[:, 9 * TT:9 * TT + 1056].rearrange("p (c t) -> p c t", c=2)
    TB = Yf[:, 12 * TT:12 * TT + 1056].rearrange("p (c t) -> p c t", c=2)
    tTA = tY[9:12]
    tTB = tY[12:15]
    DD = MIX[:, 8:16, :]
    UT = Y
    WSall = A("WS", [128, NSLAB, KC, 128], F32R)
    WS = [WSall[:, i] for i in range(NSLAB)]
    tWS = [T(f"WS{i}") for i in range(NSLAB)]
    XH = A("XH", [128, KC, 16], F32R)
    ICNT = A("ICNT", [128, 4, 16], F32)
    PSC = A("PSC", [128, 8], F32)
    LNGB = A("LNGB", [128, 10, KC], F32)
    SWTm = A("SWTm", [128, 8, 128], F32R)
    SBB = A("SBB", [128, 8, 128], F32)
    TRIU = A("TRIU", [128, 128], F32)
    RT = A("RT", [128, KC, 8], F32)
    IDENT = A("IDENT", [128, 128], F32)
    IDENTR = A("IDENTR", [128, 128], F32R)
    ONESM = A("ONESM", [128, 128], F32R)
    tC = T("constB")
    EPSB = A("EPSB", [128, 1], F32)
    PH = A("PH", [128, 8, 16], F32)
    tPH = T("PH")
    SQ = [A(f"SQ{i}", [128, TT], F32R) for i in range(2)]
    tSQ = [T(f"SQ{i}") for i in range(2)]
    MEAN = A("MEAN", [128, TT], F32)
    RSTD = A("RSTD", [128, TT], F32)
    tST = T("stats")
    NTMP = 3
    TMP = [A(f"TMP{i}", [128, TT], F32) for i in range(NTMP)]
    tTMP = [T(f"TMP{i}") for i in range(NTMP)]
    GBE = [A(f"GBE{i}", [128, TT], F32) for i in range(2)]
    tGBE = [T(f"GBE{i}") for i in range(2)]
    VTS = [A(f"VTS{i}", [128, TT], F32R) for i in range(2)]
    tVTS = [T(f"VTS{i}") for i in range(2)]
    SM = A("SM", [128, 64], F32)
    tSM = T("SM")
    LG = A("LG", [128, 4, 8], F32)
    GT = A("GT", [128, 4, 8], F32)
    M8 = A("M8", [128, 4, 8], F32)
    DG = [A(f"DG{i}", [128, 128], F32) for i in range(2)]
    tDG = [T(f"DG{i}") for i in range(2)]
    tLG = T("LG")

    SWT = Yf[:, 0:1024].rearrange("p (g t) -> p g t", g=8)
    cd = s.dsem("constB")
    if gather is not None:
        SELM = A("SELM", [128, 8, 128], BF16)
        Ybf = Y[:].bitcast(BF16)
        s.dma("sp", SELM[:], gather[2], writes=[tC], dsem=cd)
    for dst, src in ((XH[:], "xh"), (ICNT[:], "icnt"), (PSC[:], "pool_scale"), (LNGB[:], "ln_gb"),
                     (SWT, "sgu_wT"), (SBB[:], "sgu_bb"), (TRIU[:], "triu"), (RT[:], "router"),
                     (IDENT[:], "ident"), (IDENTR[:], "identr"), (ONESM[:], "onesm")):
        s.dma("sp", dst, io[src], writes=[tC] + tY[0:2], dsem=cd)
    s.op("pool", lambda: nc.gpsimd.memset(EPSB[:], EPS), reads=[], writes=[tC])
    for g in range(8):
        s.op("pool", lambda: nc.gpsimd.tensor_tensor(SWTm[:, g, :], SWT[:, g, :], TRIU[:], op=ALU.mult),
             reads=[tC] + tY[0:2], writes=[tC])

    st = dict(pb=0, ws=0, tmp=0, sq=0, dg=0, gbe=0, vts=0)

    def pp_tiles(ch):
        return tY[(ch * 2112) // 2048:((ch + 1) * 2112 - 1) // 2048 + 1]

    def bank():
        b = st["pb"] % 8
        st["pb"] += 1
        return b

    def load_slab(src_ap, nk=KC):
        i = st["ws"] % NSLAB
        st["ws"] += 1
        s.dma("sp", WS[i][:, 0:nk, :], src_ap, writes=[tWS[i]])
        return i

    def tmp():
        i = st["tmp"] % NTMP
        st["tmp"] += 1
        return i

    def mm_group(b, out_ap, pairs, reads):
        s.need("pe", reads=reads, writes=[tPS[b]])
        n = len(pairs)
        for i, (l, r) in enumerate(pairs):
            ins = nc.tensor.matmul(out_ap, l, r, start=(i == 0), stop=(i == n - 1))
        s.done("pe", ins, reads=reads, writes=[tPS[b]])

    def proj_chunk(w_ap, RHS, tRHS, nk=KC):
        sl = load_slab(w_ap, nk)
        b = bank()
        mm_group(b, PS[b][:], [(WS[sl][:, k, :], RHS[:, k, :]) for k in range(nk)], reads=[tWS[sl]] + list(tRHS[0:nk]))
        return b

    def layer_norm(gi, IN, INr, tIN, OUT, tOUT):
        b1 = bank()
        if INr is None:
            mm_group(b1, PS[b1][:], [(ONESM[:].bitcast(F32), IN[:, k, :]) for k in range(KC)], reads=list(tIN) + [tC])
        else:
            mm_group(b1, PS[b1][:], [(ONESM[:], INr[:, k, :]) for k in range(KC)], reads=list(tIN) + [tC])
        b2 = bank()
        s.need("pe", reads=[tC], writes=[tPS[b2]])
        for k in range(KC):
            q = st["sq"] % 2
            st["sq"] += 1
            s.op("act", lambda: nc.scalar.activation(out=SQ[q][:], in_=IN[:, k, :], func=AF.Square),
                 reads=[tIN[k]], writes=[tSQ[q]])
            s.op("pe", lambda: nc.tensor.matmul(PS[b2][:], ONESM[:], SQ[q][:], start=(k == 0), stop=(k == KC - 1)),
                 reads=[tSQ[q], tC], writes=[tPS[b2]] if k == KC - 1 else [])
        tm = tmp()
        s.op("dve", lambda: nc.vector.tensor_scalar(MEAN[:], PS[b1][:], 1.0 / D, None, op0=ALU.mult),
             reads=[tPS[b1]], writes=[tST])
        s.op("dve", lambda: nc.vector.tensor_tensor(TMP[tm][:], MEAN[:], MEAN[:], op=ALU.mult), reads=[tST], writes=[tTMP[tm]])
        s.op("dve", lambda: nc.vector.scalar_tensor_tensor(RSTD[:], PS[b2][:], 1.0 / D, TMP[tm][:], op0=ALU.mult, op1=ALU.subtract),
             reads=[tPS[b2], tTMP[tm]], writes=[tST])
        s.op("act", lambda: nc.scalar.activation(out=RSTD[:], in_=RSTD[:], func=AF.Sqrt, bias=EPSB[:]), reads=[tST, tC], writes=[tST])
        s.op("dve", lambda: nc.vector.reciprocal(RSTD[:], RSTD[:]), reads=[tST], writes=[tST])
        for k in range(KC):
            t1 = tmp()
            s.op("dve", lambda: nc.vector.tensor_tensor(TMP[t1][:], IN[:, k, :], MEAN[:], op=ALU.subtract),
                 reads=[tIN[k], tST], writes=[tTMP[t1]])
            s.op("pool", lambda: nc.gpsimd.tensor_tensor(TMP[t1][:], TMP[t1][:], RSTD[:], op=ALU.mult),
                 reads=[tST, tTMP[t1]], writes=[tTMP[t1]])
            s.op("act", lambda: nc.scalar.activation(out=OUT[:, k, :], in_=TMP[t1][:], func=AF.Identity,
                                                     scale=LNGB[:, gi, k:k + 1], bias=LNGB[:, gi + 1, k:k + 1]),
                 reads=[tTMP[t1], tC], writes=[tOUT[k]])

    def out_proj(w_name):
        for m in range(KC):
            b = proj_chunk(io[w_name][m], MIX, tMIX)
            s.op("dve", lambda: nc.vector.scalar_tensor_tensor(Y[:, m, :], H[:, m, :].bitcast(F32), ALPHA, PS[b][:],
                                                               op0=ALU.mult, op1=ALU.add),
                 reads=[tH[m], tPS[b]], writes=[tY[m]])

    def down_proj(w_name, slab0, first, final):
        for m in range(KC):
            b = proj_chunk(io[w_name][slab0 + m], SC3r, tSCR, nk=11)
            yo = Y[:, m, :]
            if first:
                s.op("dve", lambda: nc.vector.scalar_tensor_tensor(yo, H[:, m, :].bitcast(F32), ALPHA, PS[b][:],
                                                                   op0=ALU.mult, op1=ALU.add),
                     reads=[tH[m], tPS[b]], writes=[tY[m]])
            else:
                s.op("dve", lambda: nc.vector.tensor_tensor(yo, Y[:, m, :], PS[b][:], op=ALU.add),
                     reads=[tY[m], tPS[b]], writes=[tY[m]])

    def gu_chunk(w, idx, c, gate=None):
        bg = proj_chunk(w[2 * idx], H, tH)
        bu = proj_chunk(w[2 * idx + 1], H, tH)
        t1 = tmp()
        s.op("act", lambda: nc.scalar.activation(out=TMP[t1][:], in_=PS[bg][:], func=AF.Silu),
             reads=[tPS[bg]], writes=[tTMP[t1]])
        if gate is None:
            s.op("dve", lambda: nc.vector.tensor_tensor(SC3r[:, c, :], TMP[t1][:], PS[bu][:], op=ALU.mult),
                 reads=[tTMP[t1], tPS[bu]], writes=[tSCR[c]])
        else:
            s.op("dve", lambda: nc.vector.tensor_tensor(TMP[t1][:], TMP[t1][:], PS[bu][:], op=ALU.mult),
                 reads=[tTMP[t1], tPS[bu]], writes=[tTMP[t1]])
            s.op("pool", lambda: nc.gpsimd.tensor_tensor(SC3r[:, c, :], TMP[t1][:], GBE[gate][:], op=ALU.mult),
                 reads=[tTMP[t1], tGBE[gate]], writes=[tSCR[c]])

    xsrc = io["xTm"].rearrange("(kc p) t -> p kc t", p=128)
    if gather is None:
        asrc = attnT.rearrange("(kc p) t -> p kc t", p=128)
    else:
        gout, tG, selm = gather
        gv = gout.rearrange("(b r h p) (q t) -> p r h b q t", b=2, r=4, h=2, p=128, q=4)
    osrc = outT.rearrange("(kc p) t -> p kc t", p=128)

    for ti in range(NTILE):
        tsl = slice(ti * TT, (ti + 1) * TT)
        s.dma_multi("sp", [(H[:, c4 * 4:(c4 + 1) * 4, :], xsrc[:, c4 * 4:(c4 + 1) * 4, tsl]) for c4 in range(4)],
                    writes=tH, dsem=_ds(s, "H"))
        for ch in range(8):
            sl = load_slab(io["e_win_p"][ch])
            b = bank()
            mm_group(b, PS[b][:], [(WS[sl][:, k, :], H[:, k, :]) for k in range(KC)], reads=[tWS[sl]] + tH)
            s.op("act", lambda: nc.scalar.activation(out=PP[:, ch, 16:528], in_=PS[b][:], func=AF.Copy),
                 reads=[tPS[b]], writes=pp_tiles(ch))
            if ti == 0:
                b2 = bank()
                mm_group(b2, PS[b2][:, 0:16], [(WS[sl][:, k, :], XH[:, k, :]) for k in range(KC)], reads=[tWS[sl], tC])
                s.op("act", lambda: nc.scalar.activation(out=PP[:, ch, 0:16], in_=PS[b2][:, 0:16], func=AF.Copy),
                     reads=[tPS[b2]], writes=pp_tiles(ch))
        if ti > 0:
            s.op("pool", lambda: nc.gpsimd.tensor_copy(PP[:, :, 0:16], PH[:]), reads=[tPH], writes=tY[0:9])
        if ti < NTILE - 1:
            s.op("pool", lambda: nc.gpsimd.tensor_copy(PH[:], PP[:, :, 512:528]), reads=tY[0:9], writes=[tPH])
        for g, w in enumerate(POOL_W):
            Pg = PP[:, 2 * g:2 * g + 2, :]
            cur = Pg
            tcur = tY[0:9]
            sh = 1
            bufs = [(TA, tTA), (TB, tTB)]
            bi = 0
            lo = 0
            while sh < w:
                dst, tdst = bufs[bi % 2]
                bi += 1
                nlo = lo + sh
                eng = "pool" if (g + bi) % 2 == 0 else "dve"
                e = nc.gpsimd if eng == "pool" else nc.vector
                s.op(eng, lambda: e.tensor_tensor(dst[:, :, nlo:528], cur[:, :, nlo:528], cur[:, :, nlo - sh:528 - sh], op=ALU.add),
                     reads=list(tcur), writes=list(tdst))
                cur, tcur, lo = dst, tdst, nlo
                sh *= 2
            s.op("dve", lambda: nc.vector.scalar_tensor_tensor(DD[:, 2 * g:2 * g + 2, :], cur[:, :, 16:528], 1.0 / w, Pg[:, :, 16:528],
                                                               op0=ALU.mult, op1=ALU.subtract),
                 reads=list(tcur) + tY[0:9], writes=tMIX[8 + 2 * g:8 + 2 * g + 2])
            if ti == 0:
                for hh in range(2):
                    s.op("dve", lambda: nc.vector.tensor_tensor(TMP[0][:, 0:16], cur[:, hh, 16:32], ICNT[:, g, :], op=ALU.mult),
                         reads=list(tcur) + [tC], writes=[tTMP[0]])
                    s.op("dve", lambda: nc.vector.tensor_tensor(DD[:, 2 * g + hh, 0:16], TMP[0][:, 0:16], Pg[:, hh, 16:32], op=ALU.subtract),
                         reads=[tTMP[0]] + tY[0:9], writes=[tMIX[8 + 2 * g + hh]])
        slp = load_slab(io["pool_w"])
        for g in range(4):
            for mo in range(2):
                ch = 2 * g + mo
                b = bank()
                mm_group(b, PS[b][:], [(WS[slp][:, g * 4 + ki * 2 + mo, :], DD[:, 2 * g + ki, :]) for ki in range(2)],
                         reads=[tWS[slp], tMIX[8 + 2 * g], tMIX[8 + 2 * g + 1]])
                s.op("act", lambda: nc.scalar.activation(out=MIX[:, ch, :], in_=PS[b][:], func=AF.Identity, scale=PSC[:, ch:ch + 1]),
                     reads=[tPS[b], tC], writes=[tMIX[ch]])
        if gather is None:
            s.dma_multi("sp", [(MIX[:, 8 + c2 * 4:8 + (c2 + 1) * 4, :], asrc[:, c2 * 4:(c2 + 1) * 4, tsl]) for c2 in range(2)],
                        writes=tMIX[8:16], dsem=_ds(s, "MIXA"))
        else:
            for c in range(8):
                hp, hl = divmod(c, 2)
                k = c % 4
                tcand = tY[4 * k:4 * k + 4]
                cand = Ybf[:, 4 * k:4 * k + 4, :].rearrange("p a (b t) -> p (a b) t", t=TT)
                dst = cand.rearrange("p (b q) t -> p b q t", b=2)
                s.dma_multi("sp", [(dst[:, bq], gv[:, hp, hl, bq, :, ti * TT:(ti + 1) * TT]) for bq in range(2)],
                            reads=[tG], writes=tcand, dsem=_ds(s, f"CAND{k}"))
                b = bank()
                mm_group(b, PS[b][:], [(SELM[:, j, :], cand[:, j, :]) for j in range(8)], reads=list(tcand) + [tC])
                s.op("act", lambda: nc.scalar.activation(out=MIX[:, 8 + c, :], in_=PS[b][:], func=AF.Copy),
                     reads=[tPS[b]], writes=[tMIX[8 + c]])
        out_proj("e_wout")
        layer_norm(0, Y, None, tY, H, tH)
        for grp in range(4):
            for c in range(11):
                gu_chunk(io["e_gu"], grp * 11 + c, c)
            down_proj("e_down", grp * 16, first=(grp == 0), final=(grp == 3))
        layer_norm(2, Y, None, tY, H, tH)
        for m in range(KC):
            b = proj_chunk(io["o_win"][16 + m], H, tH)
            s.op("act", lambda: nc.scalar.activation(out=SC3r[:, m, :], in_=PS[b][:], func=AF.Gelu_apprx_tanh),
                 reads=[tPS[b]], writes=[tSCR[m]])
        for m in range(KC):
            b = proj_chunk(io["o_win"][m], H, tH)
            s.op("act", lambda: nc.scalar.activation(out=UT[:, m, :], in_=PS[b][:], func=AF.Gelu_apprx_tanh),
                 reads=[tPS[b]], writes=[tY[m]])
        layer_norm(8, SC3, SC3r, tSCR, SC3r, tSCR)
        for g in range(8):
            for hf in range(2):
                ch = 2 * g + hf
                b = bank()
                s.need("pe", reads=[tSCR[ch], tC], writes=[tPS[b]])
                for tb in range(4):
                    ins = nc.tensor.matmul(PS[b][:, tb * 128:(tb + 1) * 128], SC3r[:, ch, tb * 128:(tb + 1) * 128], IDENTR[:],
                                           start=True, stop=True)
                s.done("pe", ins, reads=[tSCR[ch], tC], writes=[tPS[b]])
                v = st["vts"] % 2
                st["vts"] += 1
                s.op("act", lambda: nc.scalar.activation(out=VTS[v][:], in_=PS[b][:], func=AF.Copy), reads=[tPS[b]], writes=[tVTS[v]])
                b2 = bank()
                s.need("pe", reads=[tVTS[v], tC], writes=[tPS[b2]])
                for tb in range(4):
                    ins = nc.tensor.matmul(PS[b2][:, tb * 128:(tb + 1) * 128], VTS[v][:, tb * 128:(tb + 1) * 128], SWTm[:, g, :],
                                           start=True, stop=True)
                s.done("pe", ins, reads=[tVTS[v], tC], writes=[tPS[b2]])
                t1 = tmp()
                s.need("dve", reads=[tPS[b2], tC], writes=[tTMP[t1]])
                for tb in range(4):
                    ins = nc.vector.tensor_tensor(TMP[t1][:, tb * 128:(tb + 1) * 128], PS[b2][:, tb * 128:(tb + 1) * 128],
                                                  SBB[:, g, :], op=ALU.add)
                s.done("dve", ins, reads=[tPS[b2], tC], writes=[tTMP[t1]])
                s.op("pool", lambda: nc.gpsimd.tensor_tensor(MIX[:, ch, :], TMP[t1][:], UT[:, ch, :], op=ALU.mult),
                     reads=[tTMP[t1], tY[ch]], writes=[tMIX[ch]])
        out_proj("o_wout")
        layer_norm(4, Y, None, tY, H, tH)
        o = 56
        for tb in range(4):
            b = bank()
            mm_group(b, PS[b][:, 0:8], [(H[:, k, tb * 128:(tb + 1) * 128].bitcast(F32), RT[:, k, :]) for k in range(KC)],
                     reads=tH + [tC])
            s.op("dve", lambda: nc.vector.tensor_copy(LG[:, tb, :], PS[b][:, 0:8]), reads=[tPS[b]], writes=[tLG])
            s.op("dve", lambda: nc.vector.max(out=M8[:, tb, :], in_=LG[:, tb, :]), reads=[tLG], writes=[tLG])
            s.op("dve", lambda: nc.vector.tensor_tensor(SM[:, o:o + 1], M8[:, tb, 1:2], M8[:, tb, 0:1], op=ALU.subtract),
                 reads=[tLG], writes=[tSM])
            s.op("act", lambda: nc.scalar.activation(out=SM[:, o + 1:o + 2], in_=SM[:, o:o + 1], func=AF.Exp), reads=[tSM], writes=[tSM])
            s.op("dve", lambda: nc.vector.tensor_scalar(SM[:, o + 2:o + 3], SM[:, o + 1:o + 2], 1.0, None, op0=ALU.add), reads=[tSM], writes=[tSM])
            s.op("dve", lambda: nc.vector.reciprocal(SM[:, o + 2:o + 3], SM[:, o + 2:o + 3]), reads=[tSM], writes=[tSM])
            s.op("dve", lambda: nc.vector.tensor_tensor(SM[:, o + 3:o + 4], SM[:, o + 1:o + 2], SM[:, o + 2:o + 3], op=ALU.mult),
                 reads=[tSM], writes=[tSM])
            s.op("dve", lambda: nc.vector.tensor_scalar(GT[:, tb, :], LG[:, tb, :], M8[:, tb, 0:1], SM[:, o + 2:o + 3],
                                                        op0=ALU.is_equal, op1=ALU.mult), reads=[tLG, tSM], writes=[tLG])
            s.op("dve", lambda: nc.vector.tensor_scalar(LG[:, tb, :], LG[:, tb, :], M8[:, tb, 1:2], SM[:, o + 3:o + 4],
                                                        op0=ALU.is_equal, op1=ALU.mult), reads=[tLG, tSM], writes=[tLG])
            s.op("dve", lambda: nc.vector.tensor_tensor(GT[:, tb, :], GT[:, tb, :], LG[:, tb, :], op=ALU.add), reads=[tLG], writes=[tLG])
        for e in range(NE):
            b = bank()
            s.need("pe", reads=[tC], writes=[tPS[b]])
            for tb in range(4):
                q = st["dg"] % 2
                st["dg"] += 1
                s.op("dve", lambda: nc.vector.tensor_scalar(DG[q][:], IDENT[:], GT[:, tb, e:e + 1], None, op0=ALU.mult),
                     reads=[tC, tLG], writes=[tDG[q]])
                s.op("pe", lambda: nc.tensor.matmul(PS[b][:, tb * 128:(tb + 1) * 128], ONESM[:].bitcast(F32), DG[q][:], start=True, stop=True),
                     reads=[tDG[q], tC], writes=[tPS[b]] if tb == 3 else [])
            ge = st["gbe"] % 2
            st["gbe"] += 1
            s.op("act", lambda: nc.scalar.activation(out=GBE[ge][:], in_=PS[b][:], func=AF.Copy), reads=[tPS[b]], writes=[tGBE[ge]])
            for c in range(11):
                gu_chunk(io["m_gu"], e * 11 + c, c, gate=ge)
            down_proj("m_down", e * 16, first=(e == 0), final=(e == NE - 1))
        layer_norm(6, Y, None, tY, MIX, tMIX)
        s.dma_multi("sp", [(osrc[:, c4 * 4:(c4 + 1) * 4, tsl], MIX[:, c4 * 4:(c4 + 1) * 4, :].bitcast(F32)) for c4 in range(4)],
                    reads=tMIX, dsem=_ds(s, "OUT"))
    s.wait_all_dma("sp", [_ds(s, "OUT")])


def build_fused(S_len, NTOK):
    nc = bass.Bass("TRN2", target_bir_lowering=False)
    nc.dge_precook = False
    ioa = declare_attn_io(nc, S_len, 2)
    io = declare_main_io(nc, NTOK)
    selm = nc.dram_tensor("selm", [128, 8, 128], BF16, kind="ExternalInput").ap()
    outT = nc.dram_tensor("outT", [D, NTOK], F32, kind="ExternalOutput").ap()
    gin = nc.dram_tensor("gin", [256, S_len], BF16)
    gout = nc.dram_tensor("gout", [8 * 256, S_len], BF16)
    s = S(nc)
    PSt = nc.alloc_psum_tensor("psall", [128, 8, 512], F32)
    PSall = PSt[:]
    PS = [PSall[:, i, :] for i in range(8)]
    tPS = [T(f"ps{i}") for i in range(8)]
    with ExitStack() as stack:
        A1 = lambda name, shape, dt: stack.enter_context(nc.sbuf_tensor(name, list(shape), dt))
        tOS = emit_attn(nc, s, A1, ioa, gin.ap().rearrange("(h p) t -> h p t", p=128), S_len, 2, PS, tPS, BF16, PSall=PSall)
        s.need("pool", writes=tOS)
        ccsem = nc.alloc_semaphore("cc")
        s.semobj["cc"] = ccsem
        nc.gpsimd.collective_compute("AllGather", ALU.bypass, replica_groups=[list(range(8))],
                                     ins=[gin.ap()], outs=[gout.ap()]).then_inc(ccsem, 1)
        tG = T("G")
        tG.w = ("cc", 1)
        s.snap[("cc", 1)] = dict(s.known["pool"])
        s.barrier()
    A2 = lambda name, shape, dt: nc.alloc_sbuf_tensor(name, list(shape), dt)
    emit_main(nc, s, io, None, outT, NTOK, PS, tPS, A=A2, gather=(gout.ap(), tG, selm))
    print("instructions", s.ninst, "waits", s.nwait)
    return nc


def host_selm(b, tq):
    import ml_dtypes
    m = np.zeros((128, 8, 128), np.float32)
    m[:, b * 4 + tq, :] = np.eye(128, dtype=np.float32)
    return m.astype(ml_dtypes.bfloat16)


POOL_W = (2, 4, 8, 16)

def tile_kn(W):
    K, N = W.shape
    return W.reshape(K // 128, 128, N // 128, 128).transpose(2, 1, 0, 3)

def vec_pk(v):
    return v.reshape(-1, 128).T

def host_weights(p):
    f = np.float32
    o = {}
    o["e_win_p"] = tile_kn(p["even_w_in"][0][:, :1024])
    o["pool_w"] = p["even_pool_w"][0].reshape(4, 2, 128, 2, 128).transpose(2, 0, 1, 3, 4).reshape(128, 16, 128)
    o["pool_scale"] = p["even_pool_scale"][0].reshape(8, 128).T
    o["e_wout"] = tile_kn(p["even_w_out"][0])
    vs = [p["even_ln1_g"][0], p["even_ln1_b"][0], p["even_ln2_g"][0], p["even_ln2_b"][0],
          p["odd_ln1_g"][0], p["odd_ln1_b"][0], p["odd_ln2_g"][0], p["odd_ln2_b"][0],
          p["odd_sgu_ln_g"][0], p["odd_sgu_ln_b"][0]]
    o["ln_gb"] = np.stack([vec_pk(v) for v in vs], axis=1)
    gu = p["even_ffn_w_gu"][0]
    o["e_gu"] = np.stack([tile_kn(gu[:, :5632]), tile_kn(gu[:, 5632:])], axis=1).reshape(88, 128, 16, 128)
    wd = p["even_ffn_w_down"][0]
    o["e_down"] = wd.reshape(4, 11, 128, 16, 128).transpose(0, 3, 2, 1, 4).reshape(64, 128, 11, 128)
    o["o_win"] = tile_kn(p["odd_w_in"][0])
    o["sgu_wT"] = p["odd_sgu_w"][0].transpose(2, 0, 1)
    o["sgu_bb"] = np.broadcast_to(p["odd_sgu_b"][0][None], (128, 8, 128))
    o["triu"] = np.triu(np.ones((128, 128), f))
    o["o_wout"] = tile_kn(p["odd_w_out"][0])
    o["router"] = p["odd_router"][0].reshape(16, 128, 8).transpose(1, 0, 2)
    mg = p["odd_moe_w_gu"][0]
    o["m_gu"] = np.stack([np.stack([tile_kn(mg[e][:, :1408]), tile_kn(mg[e][:, 1408:])], axis=1) for e in range(8)], axis=0).reshape(176, 128, 16, 128)
    md = p["odd_moe_w_down"][0]
    o["m_down"] = md.reshape(8, 11, 128, 16, 128).transpose(0, 3, 2, 1, 4).reshape(128, 128, 11, 128)
    o["ident"] = np.eye(128, dtype=f)
    o["identr"] = np.eye(128, dtype=f)
    o["onesm"] = np.ones((128, 128), f)
    return {k: np.ascontiguousarray(v, dtype=f) for k, v in o.items()}

def host_icnt(at_start):
    ic = np.zeros((128, 4, 16), np.float32)
    for g, w in enumerate(POOL_W):
        if at_start:
            ic[:, g, :] = 1.0 / np.minimum(np.arange(16) + 1, w)
        else:
            ic[:, g, :] = 1.0 / w
    return ic

def host_xh(xprev):
    return np.ascontiguousarray(xprev.T.reshape(16, 128, 16).transpose(1, 0, 2), dtype=np.float32)


SEQ = 16384
BATCH = 2
NTOKC = 4096


def kernel(**inputs):
    p = {k: np.asarray(v, dtype=np.float32) for k, v in inputs.items()}
    x = p["x"]
    f = np.float32
    xT = [np.ascontiguousarray(x[b].T) for b in range(BATCH)]
    w_in = p["even_w_in"][0]
    consts = host_consts()
    hw = host_weights(p)
    in_maps = []
    for c in range(8):
        b, j = divmod(c, 4)
        ws = []
        for hl in range(2):
            h = 2 * j + hl
            wcat = np.concatenate([w_in[:, 1024 + h * 128:1024 + (h + 1) * 128],
                                   w_in[:, 2048 + h * 128:2048 + (h + 1) * 128],
                                   w_in[:, 3072 + h * 128:3072 + (h + 1) * 128]], axis=1)
            ws.append(wcat.reshape(KC, 128, 384).transpose(1, 0, 2))
        t0 = j * NTOKC
        m = dict(hw)
        m.update(consts)
        m["xT"] = xT[b]
        m["wqkv"] = np.ascontiguousarray(np.stack(ws, 0), dtype=f)
        m["xTm"] = np.ascontiguousarray(xT[b][:, t0:t0 + NTOKC])
        m["xh"] = host_xh(x[b, t0 - 16:t0] if j > 0 else np.zeros((16, D), f))
        m["icnt"] = host_icnt(j == 0)
        m["selm"] = host_selm(b, j)
        in_maps.append(m)
    nc = build_fused(SEQ, NTOKC)
    res = run_bass_kernel_spmd(nc, in_maps, core_ids=list(range(8)))
    out = np.empty((BATCH, SEQ, D), f)
    for c in range(8):
        b, j = divmod(c, 4)
        out[b, j * NTOKC:(j + 1) * NTOKC, :] = np.asarray(res.results[c]["outT"]).T
    return out
```
